# Optimizing a Trainium2 kernel written in Bass

```python
import jax, jax.numpy as jnp
from jax import lax
import numpy as np

D_MODEL = 1024
BATCH = 8
SEQ = 4096
DEPTH = 1

HEAD_DIM = 64
RET_HEADS = 8
DSA_HEADS = 8
RET_WIDTH = RET_HEADS * HEAD_DIM
DSA_WIDTH = DSA_HEADS * HEAD_DIM
MIX_WIDTH = RET_WIDTH + DSA_WIDTH
IDX_HEADS = 8
IDX_DIM = 64
TOPK_MAX = 256
RET_CHUNK = 128
Q_BLOCK = 128
D_FF = 2816
CONV_WIDTH = 3
ROPE_THETA = 10000.0
EPS = 1e-6
IN_SIZES = (RET_WIDTH, RET_WIDTH, RET_WIDTH, RET_WIDTH,
            DSA_WIDTH, HEAD_DIM, HEAD_DIM,
            IDX_HEADS * IDX_DIM, IDX_DIM, IDX_HEADS)
IN_WIDTH = 4 * RET_WIDTH + DSA_WIDTH + 2 * HEAD_DIM + IDX_HEADS * IDX_DIM + IDX_DIM + IDX_HEADS

kernel_name = "hymba_retention_dsa_convglu_sandwich"


def rms_norm(x, g):
    xf = x.astype(jnp.float32)
    y = xf * lax.rsqrt(jnp.mean(xf * xf, axis=-1, keepdims=True) + EPS)
    return (y * g.astype(jnp.float32)).astype(x.dtype)


def rope_tables(seq_len, dim):
    inv = ROPE_THETA ** (-jnp.arange(0, dim, 2, dtype=jnp.float32) / dim)
    ang = jnp.arange(seq_len, dtype=jnp.float32)[:, None] * inv[None, :]
    return jnp.cos(ang), jnp.sin(ang)


def apply_rope(x, cos, sin):
    shape = (cos.shape[0],) + (1,) * (x.ndim - 3) + (cos.shape[1],)
    c = cos.reshape(shape)
    s = sin.reshape(shape)
    xf = x.astype(jnp.float32)
    x1, x2 = jnp.split(xf, 2, axis=-1)
    return jnp.concatenate([x1 * c - x2 * s, x2 * c + x1 * s], axis=-1).astype(x.dtype)


def split_columns(p):
    pts, acc = [], 0
    for n in IN_SIZES[:-1]:
        acc += n
        pts.append(acc)
    return jnp.split(p, pts, axis=-1)


def retention(q, k, v, gate):
    B, S, H, d = q.shape
    C = RET_CHUNK
    N = S // C
    f32 = jnp.float32
    log_g = jnp.log(1.0 - 2.0 ** (-5.0 - jnp.arange(H, dtype=f32)))
    qf = q.astype(f32).reshape(B, N, C, H, d)
    kf = (k.astype(f32) * d ** -0.5).reshape(B, N, C, H, d)
    vf = v.astype(f32).reshape(B, N, C, H, d)
    pos = jnp.arange(C, dtype=f32)
    diff = pos[:, None] - pos[None, :]
    decay_in = jnp.where(diff >= 0, jnp.exp(log_g[:, None, None] * jnp.maximum(diff, 0.0)), 0.0)
    scores = jnp.einsum('bnchd,bnmhd->bnhcm', qf, kf) * decay_in[None, None]
    inner = jnp.einsum('bnhcm,bnmhd->bnchd', scores, vf)
    zeta = jnp.exp(log_g[:, None] * (C - 1.0 - pos)[None, :])
    chunk_state = jnp.einsum('bnmhk,hm,bnmhv->bnhkv', kf, zeta, vf)
    chunk_decay = jnp.exp(log_g * C)[None, :, None, None]

    def step(R, s):
        return chunk_decay * R + s, R

    _, prev = lax.scan(step, jnp.zeros((B, H, d, d), f32), jnp.moveaxis(chunk_state, 1, 0))
    prev = jnp.moveaxis(prev, 0, 1)
    xi = jnp.exp(log_g[:, None] * (pos + 1.0)[None, :])
    cross = jnp.einsum('bnchk,bnhkv,hc->bnchv', qf, prev, xi)
    o = (inner + cross).reshape(B, S, H, d)
    o = o * lax.rsqrt(jnp.mean(o * o, axis=-1, keepdims=True) + EPS)
    o = o * jax.nn.silu(gate.astype(f32).reshape(B, S, H, d))
    return o.reshape(B, S, H * d).astype(q.dtype)


def dsa_attention(q, k, v, qi, ki, wi):
    B, S, H, d = q.shape
    k_sel = min(TOPK_MAX, S // 4)
    NB = S // Q_BLOCK
    wi = wi * (IDX_HEADS ** -0.5 * IDX_DIM ** -0.5)

    def to_blocks(a):
        return jnp.moveaxis(a.reshape((B, NB, Q_BLOCK) + a.shape[2:]), 1, 0)

    key_pos = jnp.arange(S, dtype=jnp.int32)

    def block_fn(args):
        qb, qib, wib, blk = args
        t = blk * Q_BLOCK + jnp.arange(Q_BLOCK, dtype=jnp.int32)
        rel = jax.nn.relu(jnp.einsum('bthe,bse->bths', qib, ki))
        iscore = jnp.einsum('bths,bth->bts', rel, wib).astype(jnp.float32)
        causal = key_pos[None, :] <= t[:, None]
        iscore = jnp.where(causal[None], iscore, -jnp.inf)
        _, sel = lax.top_k(iscore, k_sel)
        valid = sel <= t[None, :, None]
        k_g = jax.vmap(lambda kb, ib: kb[ib])(k, sel)
        v_g = jax.vmap(lambda vb, ib: vb[ib])(v, sel)
        logits = jnp.einsum('bthd,btkd->bhtk', qb, k_g).astype(jnp.float32) * d ** -0.5
        logits = jnp.where(valid[:, None], logits, -1e30)
        p = jax.nn.softmax(logits, axis=-1).astype(v.dtype)
        return jnp.einsum('bhtk,btkd->bthd', p, v_g)

    out = lax.map(block_fn, (to_blocks(q), to_blocks(qi), to_blocks(wi),
                             jnp.arange(NB, dtype=jnp.int32)))
    return jnp.moveaxis(out, 0, 1).reshape(B, S, H * d)


def causal_dwconv(h, w, b):
    S = h.shape[1]
    hp = jnp.pad(h, ((0, 0), (CONV_WIDTH - 1, 0), (0, 0)))
    y = b
    for j in range(CONV_WIDTH):
        y = y + hp[:, j:j + S] * w[j]
    return y


def setup_inputs(seed: int = 0) -> dict:
    key = jax.random.key(seed)
    ks = jax.random.split(key, 11)
    f32 = jnp.float32
    nrm = lambda k, shape, scale: jax.random.normal(k, shape, f32) * scale
    gain = lambda k: 1.0 + 0.02 * jax.random.normal(k, (DEPTH, D_MODEL), f32)
    return {
        "x": jax.random.normal(ks[0], (BATCH, SEQ, D_MODEL), f32),
        "mix_norm_pre": gain(ks[1]),
        "mix_norm_post": gain(ks[2]),
        "w_in": nrm(ks[3], (DEPTH, D_MODEL, IN_WIDTH), D_MODEL ** -0.5),
        "w_out": nrm(ks[4], (DEPTH, MIX_WIDTH, D_MODEL), MIX_WIDTH ** -0.5),
        "ffn_norm_pre": gain(ks[5]),
        "ffn_norm_post": gain(ks[6]),
        "w_up": nrm(ks[7], (DEPTH, D_MODEL, 2 * D_FF), D_MODEL ** -0.5),
        "conv_w": nrm(ks[8], (DEPTH, CONV_WIDTH, 2 * D_FF), CONV_WIDTH ** -0.5),
        "conv_b": nrm(ks[9], (DEPTH, 2 * D_FF), 0.01),
        "w_down": nrm(ks[10], (DEPTH, D_FF, D_MODEL), D_FF ** -0.5),
    }


def reference(x, mix_norm_pre, mix_norm_post, w_in, w_out, ffn_norm_pre, ffn_norm_post,
              w_up, conv_w, conv_b, w_down):
    B, S, _ = x.shape
    cos_h, sin_h = rope_tables(S, HEAD_DIM)
    cos_i, sin_i = rope_tables(S, IDX_DIM)
    h = x
    for l in range(DEPTH):
        hn = rms_norm(h, mix_norm_pre[l])
        proj = hn @ w_in[l]
        rq, rk, rv, rg, aq, ak, av, iq, ik, iw = split_columns(proj)
        rq = apply_rope(rq.reshape(B, S, RET_HEADS, HEAD_DIM), cos_h, sin_h)
        rk = apply_rope(rk.reshape(B, S, RET_HEADS, HEAD_DIM), cos_h, sin_h)
        rv = rv.reshape(B, S, RET_HEADS, HEAD_DIM)
        ret_out = retention(rq, rk, rv, rg)
        aq = apply_rope(aq.reshape(B, S, DSA_HEADS, HEAD_DIM), cos_h, sin_h)
        ak = apply_rope(ak, cos_h, sin_h)
        iq = apply_rope(iq.reshape(B, S, IDX_HEADS, IDX_DIM), cos_i, sin_i)
        ik = apply_rope(ik, cos_i, sin_i)
        att_out = dsa_attention(aq, ak, av, iq, ik, iw)
        mixed = jnp.concatenate([ret_out, att_out], axis=-1) @ w_out[l]
        h = h + rms_norm(mixed, mix_norm_post[l])
        hn = rms_norm(h, ffn_norm_pre[l])
        up = causal_dwconv(hn @ w_up[l], conv_w[l], conv_b[l])
        g, u = jnp.split(up, 2, axis=-1)
        ffn = (jax.nn.silu(g) * u) @ w_down[l]
        h = h + rms_norm(ffn, ffn_norm_post[l])
    return h
```

```python
from contextlib import ExitStack
import numpy as np
import ml_dtypes

import concourse.bass as bass
import concourse.mybir as mybir
from concourse.bass_utils import run_bass_kernel_spmd

F32 = mybir.dt.float32
BF16 = mybir.dt.bfloat16
AF = mybir.ActivationFunctionType
ALU = mybir.AluOpType
AX = mybir.AxisListType

D = 1024
KD = 8
HD = 64
NH = 8
IN_W = 3272
D_FF = 2816
NFC = 22
EPS = 1e-6
TOPK = 256
N_BISECT = 13

C_RQ, C_RK, C_AQ, C_IQ = 0, 512, 1024, 1536
C_AK, C_IK = 2048, 2112
C_RV, C_RG, C_AV, C_IW = 2176, 2688, 3200, 3264


class Prog:
    CE = ("pe", "act", "dve", "pool")

    def __init__(self, nc, es):
        self.nc = nc
        self.es = es
        self.lists = {e: [] for e in ("pe", "act", "dve", "pool", "sp")}
        self.cnt = {e: 0 for e in self.CE}
        self.psem = {e: es.enter_context(nc.semaphore("prog_" + e)) for e in self.CE}
        self.dstream = {}
        self.res = {}
        self.seen = {e: {} for e in self.lists}

    def _stream(self, name, group=False):
        if name not in self.dstream:
            self.dstream[name] = [self.es.enter_context(self.nc.semaphore("dq_" + name)), 0, group]
        return self.dstream[name]

    def op(self, eng, fn, reads=(), writes=(), dma=None, group=False):
        waits = {}
        if dma is not None:
            st = self._stream(dma, group)
            if not st[2] and st[1] > 0:
                waits[("d", dma)] = st[1]

        def need(tok, kind):
            if tok is None:
                return
            tk, who, val = tok
            if tk == "c":
                if who == eng and dma is None:
                    if eng == "pe":
                        return
                key = ("c", who)
                waits[key] = max(waits.get(key, 0), val)
            else:
                key = ("d", who)
                waits[key] = max(waits.get(key, 0), self.dstream[who][1])

        for r in reads:
            ent = self.res.get(r)
            if ent:
                need(ent[0], "raw")
        for w in writes:
            ent = self.res.get(w)
            if ent:
                need(ent[0], "waw")
                for tok in ent[1].values():
                    need(tok, "war")
        if dma is not None:
            st = self._stream(dma)
            st[1] += 16
            tok = ("d", dma, st[1])
            inc = (st[0], 16)
        else:
            self.cnt[eng] += 1
            tok = ("c", eng, self.cnt[eng])
            inc = (self.psem[eng], 1)
        for r in reads:
            ent = self.res.setdefault(r, [None, {}])
            ent[1][(tok[0], tok[1])] = tok
        for w in writes:
            self.res[w] = [tok, {}]
        wl = []
        seen = self.seen[eng]
        for key, val in waits.items():
            if seen.get(key, 0) >= val:
                continue
            seen[key] = val
            wl.append((key, val))
        self.lists[eng].append((fn, wl, inc))

    def barrier(self):
        for eng in self.lists:
            wl = [(("c", e), self.cnt[e]) for e in self.CE if e != eng and self.cnt[e] > 0]
            wl += [(("d", s), st[1]) for s, st in self.dstream.items() if st[1] > 0]
            self.lists[eng].append((None, wl, None))
            for key, val in wl:
                self.seen[eng][key] = max(self.seen[eng].get(key, 0), val)

    def final_wait(self, eng, streams):
        wl = [(("d", s), self.dstream[s][1]) for s in streams if s in self.dstream]
        self.lists[eng].append((None, wl, None))

    def emit(self):
        nc = self.nc
        lists = self.lists

        def run(e, items):
            for fn, wl, inc in items:
                for key, val in wl:
                    if key[0] == "c":
                        sem = self.psem[key[1]]
                    else:
                        st = self.dstream[key[1]]
                        sem = st[0]
                        if st[2]:
                            val = st[1]
                    e.wait_ge(sem, val)
                if fn is not None:
                    ins = fn(e)
                    ins.then_inc(inc[0], inc[1])

        self.lists = {e: [] for e in lists}
        with nc.Block() as block:
            @block.tensor
            def _(e):
                run(e, lists["pe"])

            @block.scalar
            def _(e):
                run(e, lists["act"])

            @block.vector
            def _(e):
                run(e, lists["dve"])

            @block.gpsimd
            def _(e):
                run(e, lists["pool"])

            @block.sync
            def _(e):
                run(e, lists["sp"])


def build_nc(S, do_mixer=True, do_ffn=True, dbg=False):
    NT = S // 128
    TS2 = 256
    NST = S // TS2
    nc = bass.Bass("TRN2", target_bir_lowering=False)
    es = ExitStack()

    def din(name, shape, dt=F32):
        return nc.dram_tensor(name, list(shape), dt, kind="ExternalInput").ap()

    x = din("x", [S, D])
    w_in = din("w_in", [D, IN_W])
    w_out = din("w_out", [D, D])
    w_up = din("w_up", [D, 2 * D_FF])
    w_down = din("w_down", [D_FF, D])
    cwb = din("cwb", [128, 2 * NFC, 4])
    gains = din("gains", [4, D])
    cs_tab = din("cs_tab", [128, NT, 64])
    consts = din("consts", [128, 512])
    out = nc.dram_tensor("out", [S, D], F32, kind="ExternalOutput").ap()

    P = Prog(nc, es)

    def sb(name, shape, dt=F32):
        return es.enter_context(nc.sbuf_tensor(name, list(shape), dt))

    def ps(name, shape, dt=F32):
        return es.enter_context(nc.psum_tensor(name, list(shape), dt))

    ident = sb("ident", [128, 128], BF16)
    cst = sb("cst", [128, 512])
    neg_half = sb("neg_half", [128, 8])
    P.op("sp", lambda e: e.dma_start(out=cst[:], in_=consts), writes=["cst"], dma="c0")
    P.op("dve", lambda e: e.tensor_copy(out=ident[:], in_=cst[:, 0:128]), reads=["cst"], writes=["ident"])
    P.op("pool", lambda e: e.memset(neg_half[:], -0.5), writes=["neg_half"])

    psA = [ps("psA0", [128, 1024], BF16), ps("psA1", [128, 512])]
    psA1v = psA[1][:].bitcast(BF16)
    psAv = [psA[0][:], psA1v]
    psB = [ps("psB%d" % i, [128, 512]) for i in range(2)]
    psC = [ps("psC%d" % i, [128, 512]) for i in range(2)]
    psD = [ps("psD%d" % i, [128, 512]) for i in range(2)]

    def rstd_from_ss(ss_ap, rstd_ap, tmp_ap, key_ss, key_tmp, key_rstd, n, inv_n):
        P.op("pool", lambda e: e.tensor_scalar(out=tmp_ap, in0=ss_ap, scalar1=inv_n, scalar2=EPS,
                                               op0=ALU.mult, op1=ALU.add),
             reads=[key_ss], writes=[key_tmp])
        P.op("pool", lambda e: e.tensor_tensor(out=rstd_ap, in0=tmp_ap, in1=neg_half[:, 0:n], op=ALU.pow),
             reads=[key_tmp, "neg_half"], writes=[key_rstd])

    h1_src = out if do_mixer else x
    TOPK_ = min(TOPK, S // 4)
    NB = N_BISECT

    if do_mixer:
        es1 = ExitStack()

        def sb1(name, shape, dt=F32):
            return es1.enter_context(nc.sbuf_tensor(name, list(shape), dt))

        dbg_mix = None
        if dbg:
            dbg_mix = nc.dram_tensor("dbg_mix", [S, D], BF16, kind="ExternalOutput").ap()
        gpost = sb1("gpost", [128, D])
        P.op("sp", lambda e: e.dma_start(out=gpost[:], in_=gains[1, :].partition_broadcast(128)),
             writes=["gbc"], dma="c1")

        w_in_sb = sb1("w_in_sb", [128, KD, IN_W], BF16)
        w_out_sb = sb1("w_out_sb", [128, KD, D], BF16)
        cs2 = [sb1("cs%d" % k, [128, 64]) for k in range(2)]
        xb = [sb1("xb%d" % k, [128, D]) for k in range(4)]
        xn_bf = sb1("xn_bf", [128, D], BF16)
        junk1 = sb1("junk1", [128, D], BF16)
        junk2 = junk1
        xnT = sb1("xnT", [128, KD, 128], BF16)
        qk_tok = sb1("qk_tok", [128, 2304], BF16)
        rt = sb1("rt", [128, 1024])
        vtok = sb1("vtok", [128, NH, HD], BF16)
        g_s = sb1("g_s", [128, 512])
        g_e = sb1("g_e", [128, 512])
        g_sg = g_e
        rqkT = sb1("rqkT", [128, 8, 128], BF16)
        aqT3 = [sb1("aqT%d" % k, [128, 4, 128], BF16) for k in range(3)]
        iqTz = sb1("iqTz", [128, NH, 128], BF16)
        akT2 = sb1("akT2", [128, S], BF16)
        ikT2 = sb1("ikT2", [128, S], BF16)
        av1 = sb1("av1", [128, NT, 65], BF16)
        iwb = sb1("iwb", [128, 8])
        scT = sb1("scT", [128, 2, 4, 128], BF16)
        R32 = sb1("R32", [128, 4, HD])
        Rbf = sb1("Rbf", [128, 4, HD], BF16)
        osq = sb1("osq", [128, 2, 4, HD])
        st8 = sb1("st8", [128, 8, 8])
        iscore2 = [sb1("iscore%d" % k, [128, S]) for k in range(2)]
        relb = [sb1("relb%d" % k, [128, 512], BF16) for k in range(4)]
        diagw = sb1("diagw", [128, NH, 128], BF16)
        maskb_ = sb1("maskb", [128, S], BF16)
        maskb2 = [maskb_, maskb_]
        maskT = sb1("maskT", [128, NT, 128], BF16)
        eb = [sb1("eb%d" % k, [128, 2, 4, 128], BF16) for k in range(2)]
        ret3 = [sb1("ret%d" % k, [128, 512], BF16) for k in range(3)]
        att1 = sb1("att1", [128, 512], BF16)
        mixT = sb1("mixT", [128, KD, 128], BF16)
        bs = sb1("bs", [128, 8 + NB])
        sm = sb1("sm", [128, 16])

        maskT01 = cst[:, 128:256]
        negmask = cst[:, 256:384]
        kscale = cst[:, 384:392]
        xi2_64 = cst[:, 392:400]
        xi_pp = cst[:, 400:408]
        gC = cst[:, 408:412]
        pow2 = cst[:, 412:412 + NB]

        P.op("sp", lambda e: e.dma_start(out=cs2[0][:], in_=cs_tab[:, 0, :]), writes=["cs0"], dma="csld0")
        win_v = w_in.rearrange("(kc p) f -> p kc f", p=128)
        bounds = [0, 512, 1024, 1536, 2048, 2176, 2688, 3200, IN_W]
        for k in range(8):
            a, b = bounds[k], bounds[k + 1]
            P.op("pool", lambda e, a=a, b=b: e.dma_start(out=w_in_sb[:, :, a:b], in_=win_v[:, :, a:b]),
                 writes=["w_in%d" % k], dma="win%d" % k)
            P.op("dve", lambda e, a=a, b=b: e.tensor_tensor(
                out=w_in_sb[:, :, a:b], in0=w_in_sb[:, :, a:b],
                in1=cst[:, 440:448].unsqueeze(2).broadcast_to([128, KD, b - a]), op=ALU.mult),
                 reads=["w_in%d" % k, "cst"], writes=["w_in%d" % k])
        wout_v = w_out.rearrange("(kc p) f -> p kc f", p=128)
        for k in range(2):
            P.op("pool", lambda e, k=k: e.dma_start(out=w_out_sb[:, :, k * 512:(k + 1) * 512],
                                                    in_=wout_v[:, :, k * 512:(k + 1) * 512]),
                 writes=["w_out%d" % k], dma="wout", group=True)
        P.op("pool", lambda e: e.memset(av1[:, :, 64:65], 1.0), writes=["av1_ones"])
        P.op("pool", lambda e: e.memset(R32[:], 0.0), writes=["R32"])
        P.op("pool", lambda e: e.memset(iqTz[:], 0.0), writes=["iqT"])
        P.op("sp", lambda e: e.dma_start(out=xb[0][:], in_=x[0:128, :]), writes=["xb0"], dma="xld0")

        frot = [0]

        def next_bank():
            b = (psB[frot[0] % 2], "psB%d" % (frot[0] % 2))
            frot[0] += 1
            return b

        def front(i):
            xbuf, xk = xb[i % 4], "xb%d" % (i % 4)
            aqT, aqk = aqT3[i % 3], "aqT%d" % (i % 3)
            ret, retk = ret3[i % 3], "ret%d" % (i % 3)
            cs, csk = cs2[i % 2], "cs%d" % (i % 2)
            if i + 1 < NT:
                P.op("sp", lambda e, i=i: e.dma_start(out=cs2[(i + 1) % 2][:], in_=cs_tab[:, i + 1, :]),
                     writes=["cs%d" % ((i + 1) % 2)], dma="csld%d" % ((i + 1) % 2))
                nb_, nk_ = xb[(i + 1) % 4], "xb%d" % ((i + 1) % 4)
                P.op("sp", lambda e, nb_=nb_, i=i: e.dma_start(out=nb_[:], in_=x[(i + 1) * 128:(i + 2) * 128, :]),
                     writes=[nk_], dma="xld%d" % ((i + 1) % 4))
            P.op("act", lambda e, xbuf=xbuf: e.activation(out=junk1[:], in_=xbuf[:], func=AF.Square,
                                                          accum_out=sm[:, 0:1]),
                 reads=[xk], writes=["junk1", "sm0"])
            rstd_from_ss(sm[:, 0:1], sm[:, 2:3], sm[:, 1:2], "sm0", "sm1", "sm2", 1, 1.0 / D)
            P.op("dve", lambda e, xbuf=xbuf: e.tensor_scalar(
                out=xn_bf[:], in0=xbuf[:], scalar1=sm[:, 2:3], scalar2=None, op0=ALU.mult),
                 reads=[xk, "sm2"], writes=["xn_bf"])
            for kc in range(KD):
                P.op("pe", lambda e, kc=kc: e.transpose(out=psA[0][:, kc * 128:(kc + 1) * 128],
                                                        in_=xn_bf[:, kc * 128:(kc + 1) * 128], identity=ident[:]),
                     reads=["xn_bf", "ident"], writes=["psA0"])
            P.op("act", lambda e: e.copy(out=xnT[:], in_=psA[0][:].rearrange("p (k t) -> p k t", k=KD)),
                 reads=["psA0"], writes=["xnT"])
            yield

            def proj_slice(k):
                a, b = bounds[k], bounds[k + 1]
                pb, pbk = next_bank()
                for kc in range(KD):
                    P.op("pe", lambda e, kc=kc, pb=pb, a=a, b=b: e.matmul(
                        pb[:, 0:b - a], lhsT=xnT[:, kc, :], rhs=w_in_sb[:, kc, a:b],
                        start=(kc == 0), stop=(kc == KD - 1)),
                         reads=["xnT", "w_in%d" % k], writes=[pbk])
                return pb, pbk

            def rope(pb, pbk, nh, dst3, dkey):
                P.op("act", lambda e: e.copy(out=rt[:, 0:nh * 64], in_=pb[:, 0:nh * 64]), reads=[pbk], writes=["rt_x"])
                src = rt[:, 0:nh * 64].rearrange("p (h two f) -> p h two f", two=2, f=32)
                x1, x2 = src[:, :, 0, :], src[:, :, 1, :]
                c = cs[:, 0:32].unsqueeze(1).broadcast_to([128, nh, 32])
                sn = cs[:, 32:64].unsqueeze(1).broadcast_to([128, nh, 32])
                ta = rt[:, 512:512 + nh * 32].rearrange("p (h f) -> p h f", f=32)
                tb = rt[:, 768:768 + nh * 32].rearrange("p (h f) -> p h f", f=32)
                for (xa, xb_, op_, dst_) in ((x1, x2, ALU.subtract, dst3[:, :, 0:32]), (x2, x1, ALU.add, dst3[:, :, 32:64])):
                    P.op("pool", lambda e, xa=xa: e.tensor_tensor(out=ta, in0=xa, in1=c, op=ALU.mult),
                         reads=["rt_x", csk], writes=["rt_a"])
                    P.op("pool", lambda e, xb_=xb_: e.tensor_tensor(out=tb, in0=xb_, in1=sn, op=ALU.mult),
                         reads=["rt_x", csk], writes=["rt_b"])
                    P.op("pool", lambda e, op_=op_, dst_=dst_: e.tensor_tensor(out=dst_, in0=ta, in1=tb, op=op_),
                         reads=["rt_a", "rt_b"], writes=[dkey])

            for k in range(4):
                pb, pbk = proj_slice(k)
                rope(pb, pbk, 8, qk_tok[:, k * 512:(k + 1) * 512].rearrange("p (h d) -> p h d", d=64), "qk_tok%d" % k)
                yield
            pb, pbk = proj_slice(4)
            kk4 = qk_tok[:, 2048:2304].rearrange("p (h r d) -> p h r d", h=2, r=2)
            rope(pb, pbk, 2, kk4[:, :, 0, :], "qk_tok4")
            P.op("pool", lambda e: e.tensor_copy(out=kk4[:, :, 1, :], in_=kk4[:, :, 0, :]),
                 reads=["qk_tok4"], writes=["qk_tok4b"])
            yield
            pb, pbk = proj_slice(5)
            P.op("act", lambda e, pb=pb: e.copy(out=rt[:, 0:512], in_=pb[:]), reads=[pbk], writes=["rt_x"])
            P.op("pool", lambda e: e.tensor_tensor(
                out=vtok[:], in0=rt[:, 0:512].rearrange("p (h d) -> p h d", d=64),
                in1=kscale.unsqueeze(2).broadcast_to([128, NH, HD]), op=ALU.mult),
                 reads=["rt_x", "cst"], writes=["vtok"])
            yield
            pb, pbk = proj_slice(6)
            P.op("act", lambda e, pb=pb: e.activation(out=g_e[:], in_=pb[:], func=AF.Tanh, scale=0.5),
                 reads=[pbk], writes=["g_e"])
            P.op("act", lambda e, pb=pb: e.copy(out=g_s[:], in_=pb[:]), reads=[pbk], writes=["g_s"])
            P.op("pool", lambda e: e.tensor_scalar(out=g_e[:], in0=g_e[:], scalar1=1.0, scalar2=1.0, op0=ALU.add,
                                                   op1=ALU.mult), reads=["g_e"], writes=["g_e"])
            P.op("pool", lambda e: e.tensor_tensor(out=g_sg[:], in0=g_s[:], in1=g_e[:], op=ALU.mult),
                 reads=["g_s", "g_e"], writes=["g_e", "g_sg"])
            yield
            pb, pbk = proj_slice(7)
            P.op("act", lambda e, pb=pb: e.copy(out=av1[:, i, 0:64], in_=pb[:, 0:64]), reads=[pbk], writes=["av1_%d" % i])
            P.op("act", lambda e, pb=pb: e.mul(out=iwb[:], in_=pb[:, 64:72], mul=float((8 ** -0.5) * (64 ** -0.5))),
                 reads=[pbk], writes=["iwb"])

            P.op("pool", lambda e: e.tensor_tensor(
                out=diagw[:], in0=ident[:].unsqueeze(1).broadcast_to([128, NH, 128]),
                in1=iwb[:].unsqueeze(2).broadcast_to([128, NH, 128]), op=ALU.mult),
                 reads=["ident", "iwb"], writes=["diagw"])
            yield
            for rnd in range(2):
                for bl in range(8):
                    gb = rnd * 8 + bl
                    P.op("pe", lambda e, bl=bl, gb=gb: e.transpose(
                        out=psA[0][:, bl * 128:(bl + 1) * 128], in_=qk_tok[:, gb * 128:(gb + 1) * 128], identity=ident[:]),
                         reads=["qk_tok%d" % (gb // 4), "ident"], writes=["psA0"])
                if rnd == 0:
                    P.op("act", lambda e: e.copy(out=rqkT[:], in_=psA[0][:].rearrange("p (k t) -> p k t", k=8)),
                         reads=["psA0"], writes=["rqkT"])
                else:
                    P.op("act", lambda e: e.copy(out=aqT[:], in_=psA[0][:, 0:512].rearrange("p (k t) -> p k t", k=4)),
                         reads=["psA0"], writes=[aqk])
                    for par in range(2):
                        P.op("act", lambda e, par=par: e.copy(
                            out=iqTz[par * 64:(par + 1) * 64].rearrange("p (b two) t -> p b two t", two=2)[:, :, par, :],
                            in_=psA[0][par * 64:(par + 1) * 64, 512:1024].rearrange("p (k t) -> p k t", k=4)),
                             reads=["psA0"], writes=["iqT"])
                yield
            for bl in range(2):
                P.op("pe", lambda e, bl=bl: e.transpose(
                    out=psA[0][:, bl * 128:(bl + 1) * 128], in_=qk_tok[:, 2048 + bl * 128:2048 + (bl + 1) * 128],
                    identity=ident[:]), reads=["qk_tok4", "qk_tok4b", "ident"], writes=["psA0"])
            P.op("act", lambda e: e.copy(out=akT2[:, i * 128:(i + 1) * 128], in_=psA[0][:, 0:128]),
                 reads=["psA0"], writes=["akT2_%d" % i])
            P.op("act", lambda e: e.copy(out=ikT2[:, i * 128:(i + 1) * 128], in_=psA[0][:, 128:256]),
                 reads=["psA0"], writes=["ikT2_%d" % i])
            yield

            scb = [(psB[0], "psB0"), (psB[1], "psB1")]
            for h in range(NH):
                par, pr = h % 2, h // 2
                pb, pbk = scb[par]
                P.op("pe", lambda e, par=par, pr=pr, pb=pb: e.matmul(
                    pb[:, pr * 128:(pr + 1) * 128], lhsT=rqkT[par * 64:(par + 1) * 64, 4 + pr, :],
                    rhs=rqkT[par * 64:(par + 1) * 64, pr, :], start=True, stop=True),
                     reads=["rqkT"], writes=[pbk])
            for par in range(2):
                pb, pbk = scb[par]
                P.op("act", lambda e, par=par, pb=pb: e.copy(out=rt[:, par * 512:(par + 1) * 512], in_=pb[:]),
                     reads=[pbk], writes=["rt_x" if par == 0 else "rt_a", "rt_b"])
                P.op("pool", lambda e, par=par: e.tensor_tensor(
                    out=scT[:, par], in0=rt[:, par * 512:(par + 1) * 512].rearrange("p (k t) -> p k t", k=4),
                    in1=maskT01.unsqueeze(1).broadcast_to([128, 4, 128]), op=ALU.mult),
                     reads=["rt_x", "rt_a", "rt_b", "cst"], writes=["scT%d" % par])
            yield
            for h in range(NH):
                par, pr = h % 2, h // 2
                P.op("pe", lambda e, par=par, pr=pr, h=h: e.matmul(
                    psB[par][:, pr * 64:(pr + 1) * 64], lhsT=scT[:, par, pr, :], rhs=vtok[:, h, :],
                    start=(pr == 0), stop=(i == 0 and pr == 3), skip_group_check=True),
                     reads=["scT%d" % par, "vtok"], writes=["psB%d" % par])
            if i > 0:
                for h in range(NH):
                    par, pr = h % 2, h // 2
                    P.op("pe", lambda e, par=par, pr=pr: e.matmul(
                        psB[par][:, pr * 64:(pr + 1) * 64], lhsT=rqkT[par * 64:(par + 1) * 64, pr, :],
                        rhs=Rbf[par * 64:(par + 1) * 64, pr, :], start=False, stop=(pr == 3),
                        skip_group_check=True),
                         reads=["rqkT", "Rbf"], writes=["psB%d" % par])
            for par in range(2):
                P.op("act", lambda e, par=par: e.activation(
                    out=osq[:, par], in_=psB[par][:, 0:256].rearrange("p (k d) -> p k d", d=64), func=AF.Square),
                     reads=["psB%d" % par], writes=["osq%d" % par])
            P.op("dve", lambda e: e.tensor_reduce(out=st8[:, 0, :], in_=osq[:].rearrange("p a k d -> p (a k) d"),
                                                  axis=AX.X, op=ALU.add),
                 reads=["osq0", "osq1"], writes=["st8_0"])
            P.op("pool", lambda e: e.tensor_tensor(out=st8[:, 1, :], in0=st8[:, 0, :], in1=xi2_64, op=ALU.mult),
                 reads=["st8_0", "cst"], writes=["st8_1"])
            rstd_from_ss(st8[:, 1, :], st8[:, 3, :], st8[:, 2, :], "st8_1", "st8_2", "st8_3", 8, 1.0)
            P.op("pool", lambda e: e.tensor_tensor(out=st8[:, 4, :], in0=st8[:, 3, :], in1=xi_pp, op=ALU.mult),
                 reads=["st8_3", "cst"], writes=["st8_4"])
            for par in range(2):
                P.op("dve", lambda e, par=par: e.tensor_tensor(
                    out=osq[:, par], in0=psB[par][:, 0:256].rearrange("p (k d) -> p k d", d=64),
                    in1=st8[:, 4, par * 4:(par + 1) * 4].unsqueeze(2).broadcast_to([128, 4, HD]), op=ALU.mult),
                     reads=["psB%d" % par, "st8_4", "osq%d" % par], writes=["osq%d" % par])
            P.op("pool", lambda e: e.tensor_tensor(
                out=ret[:].rearrange("p (k a d) -> p a k d", a=2, d=64), in0=osq[:],
                in1=g_sg[:].rearrange("p (k a d) -> p a k d", a=2, d=64), op=ALU.mult),
                 reads=["osq0", "osq1", "g_sg"], writes=[retk])
            yield
            pbS, pbSk = next_bank()
            for h in range(NH):
                par, pr = h % 2, h // 2
                P.op("pe", lambda e, par=par, pr=pr, h=h, pbS=pbS: e.matmul(
                    pbS[par * 64:(par + 1) * 64, pr * 64:(pr + 1) * 64],
                    lhsT=qk_tok[:, 512 + h * 64:512 + (h + 1) * 64], rhs=vtok[:, h, :], start=True, stop=True),
                     reads=["qk_tok1", "vtok"], writes=[pbSk])
            P.op("act", lambda e, pbS=pbS: e.copy(out=rt[:, 0:256], in_=pbS[:, 0:256]), reads=[pbSk], writes=["rt_x"])
            P.op("pool", lambda e: e.tensor_tensor(
                out=R32[:], in0=R32[:], in1=rt[:, 0:256].rearrange("p (k d) -> p k d", d=64), op=ALU.add),
                 reads=["rt_x", "R32"], writes=["R32"])
            P.op("pool", lambda e: e.tensor_tensor(out=R32[:], in0=R32[:],
                                                   in1=gC.unsqueeze(2).broadcast_to([128, 4, HD]), op=ALU.mult),
                 reads=["R32", "cst"], writes=["R32"])
            P.op("act", lambda e: e.copy(out=Rbf[:], in_=R32[:]), reads=["R32"], writes=["Rbf"])

        def indexmm(i):
            nk = (i + 1) * 128
            iscore, isk = iscore2[i % 2], "iscore%d" % (i % 2)
            ibanks = [(psB[0][:], "psB0"), (psB[1][:], "psB1"), (psA[0][:].bitcast(F32), "psA0")]
            LA = 2
            for kg in range((nk + 511) // 512):
                c0 = kg * 512
                w = min(512, nk - c0)
                acc, acck = psA[1], "psA1"

                def mm_relu(h):
                    par, pr = h % 2, h // 2
                    pb, pbk = ibanks[h % 3]
                    rb, rbk = relb[h % 4], "relb%d" % (h % 4)
                    P.op("pe", lambda e, h=h, pb=pb, c0=c0, w=w: e.matmul(
                        pb[:, 0:w], lhsT=iqTz[:, h, :], rhs=ikT2[:, c0:c0 + w], start=True, stop=True),
                         reads=["iqT"] + ["ikT2_%d" % t for t in range(c0 // 128, (c0 + w) // 128)], writes=[pbk])
                    P.op("act", lambda e, rb=rb, pb=pb, w=w: e.activation(out=rb[:, 0:w], in_=pb[:, 0:w], func=AF.Relu),
                         reads=[pbk], writes=[rbk])

                def diag(h):
                    rb, rbk = relb[h % 4], "relb%d" % (h % 4)
                    P.op("pe", lambda e, rb=rb, h=h, w=w: e.matmul(
                        psA[1][:, 0:w], lhsT=diagw[:, h, :], rhs=rb[:, 0:w], start=(h == 0), stop=(h == NH - 1)),
                         reads=[rbk, "diagw"], writes=[acck])

                for h in range(LA):
                    mm_relu(h)
                for h in range(NH):
                    diag(h)
                    if h + LA < NH:
                        mm_relu(h + LA)
                P.op("act", lambda e, c0=c0, w=w, iscore=iscore: e.copy(out=iscore[:, c0:c0 + w], in_=psA[1][:, 0:w]),
                     reads=[acck], writes=[isk])
                yield
        def chain(i):
            maskb, mbk = maskb2[i % 2], "maskb"
            iscore, isk = iscore2[i % 2], "iscore%d" % (i % 2)
            nk = (i + 1) * 128
            P.op("dve", lambda e, nk=nk: e.tensor_tensor(out=iscore[:, nk - 128:nk], in0=iscore[:, nk - 128:nk],
                                                         in1=negmask, op=ALU.add),
                 reads=[isk, "cst"], writes=[isk])
            yield
            if i * 128 >= TOPK_:
                P.op("dve", lambda e: e.tensor_reduce(out=bs[:, 0:1], in_=iscore[:, 0:TOPK_], axis=AX.X,
                                                      op=ALU.min), reads=[isk], writes=["bs0"])
                P.op("dve", lambda e, nk=nk: e.tensor_reduce(out=bs[:, 1:2], in_=iscore[:, 0:nk], axis=AX.X,
                                                             op=ALU.max), reads=[isk], writes=["bs1"])
                P.op("dve", lambda e: e.tensor_tensor(out=bs[:, 2:3], in0=bs[:, 1:2], in1=bs[:, 0:1], op=ALU.subtract),
                     reads=["bs0", "bs1"], writes=["bs2"])
                P.op("dve", lambda e: e.tensor_scalar(out=bs[:, 8:8 + NB], in0=pow2, scalar1=bs[:, 2:3], scalar2=None,
                                                      op0=ALU.mult), reads=["bs2", "cst"], writes=["bswk"])
                P.op("dve", lambda e: e.tensor_tensor(out=bs[:, 3:4], in0=bs[:, 0:1], in1=bs[:, 8:9], op=ALU.add),
                     reads=["bs0", "bswk"], writes=["bs3"])
                for k in range(NB):
                    P.op("dve", lambda e, nk=nk: e.tensor_scalar(
                        out=maskb[:, 0:nk], in0=iscore[:, 0:nk], scalar1=bs[:, 3:4], scalar2=None,
                        op0=ALU.is_ge, op1=ALU.add, accum_out=bs[:, 4:5]),
                         reads=[isk, "bs3"], writes=[mbk, "bs4"])
                    P.op("dve", lambda e: e.tensor_scalar(out=bs[:, 5:6], in0=bs[:, 4:5], scalar1=float(TOPK_) - 0.5,
                                                          scalar2=-0.5, op0=ALU.is_gt, op1=ALU.add),
                         reads=["bs4"], writes=["bs5"])
                    P.op("dve", lambda e, k=k: e.scalar_tensor_tensor(
                        out=bs[:, 3:4], in0=bs[:, 5:6], scalar=bs[:, 8 + k:9 + k], in1=bs[:, 3:4],
                        op0=ALU.mult, op1=ALU.add), reads=["bs5", "bswk", "bs3"], writes=["bs3"])
                    yield
                P.op("dve", lambda e: e.scalar_tensor_tensor(
                    out=bs[:, 6:7], in0=bs[:, 8 + NB - 1:8 + NB], scalar=-0.5, in1=bs[:, 3:4],
                    op0=ALU.mult, op1=ALU.add), reads=["bswk", "bs3"], writes=["bs6"])
                P.op("dve", lambda e, nk=nk: e.tensor_scalar(
                    out=maskb[:, 0:nk], in0=iscore[:, 0:nk], scalar1=bs[:, 6:7], scalar2=None, op0=ALU.is_ge),
                     reads=[isk, "bs6"], writes=[mbk])
            else:
                P.op("dve", lambda e, nk=nk: e.tensor_scalar(
                    out=maskb[:, 0:nk], in0=iscore[:, 0:nk], scalar1=-1e29, scalar2=None, op0=ALU.is_ge),
                     reads=[isk], writes=[mbk])

        def mask_tr(i):
            maskb, mbk = maskb2[i % 2], "maskb"
            for j0 in range(0, i + 1, 8):
                nb8 = min(8, i + 1 - j0)
                pa, pak = (psA1v, "psA1") if (j0 // 8) % 2 == 0 else (psA[0][:], "psA0")
                for bl in range(nb8):
                    j = j0 + bl
                    P.op("pe", lambda e, bl=bl, j=j, pa=pa: e.transpose(
                        out=pa[:, bl * 128:(bl + 1) * 128], in_=maskb[:, j * 128:(j + 1) * 128], identity=ident[:]),
                         reads=[mbk, "ident"], writes=[pak])
                P.op("act", lambda e, j0=j0, nb8=nb8, pa=pa: e.copy(
                    out=maskT[:, j0:j0 + nb8, :], in_=pa[:, 0:nb8 * 128].rearrange("p (k t) -> p k t", t=128)),
                     reads=[pak], writes=["maskT"])

        def attn(i):
            aqT, aqk = aqT3[i % 3], "aqT%d" % (i % 3)
            lb = [(psC[0], "psC0"), (psC[1], "psC1")]

            def stage_a(j):
                ebj, ebk = eb[j % 2], "eb%d" % (j % 2)
                ptj, ptk = eb[j % 2], "pT%d" % (j % 2)
                for par in range(2):
                    pb, pbk = lb[par]
                    P.op("pe", lambda e, par=par, pb=pb, j=j: e.matmul(
                        pb[:], lhsT=akT2[par * 64:(par + 1) * 64, j * 128:(j + 1) * 128],
                        rhs=aqT[par * 64:(par + 1) * 64, :, :], start=True, stop=True),
                         reads=[aqk, "akT2_%d" % j], writes=[pbk])
                    P.op("act", lambda e, par=par, pb=pb, ebj=ebj: e.activation(
                        out=ebj[:, par], in_=pb[:].rearrange("p (k t) -> p k t", t=128), func=AF.Exp, scale=0.125),
                         reads=[pbk], writes=[ebk + "_%d" % par])
                P.op("pool", lambda e, ebj=ebj, ptj=ptj, j=j: e.tensor_tensor(
                    out=ptj[:].rearrange("p a k t -> p (a k) t"), in0=ebj[:].rearrange("p a k t -> p (a k) t"),
                    in1=maskT[:, j, :].unsqueeze(1).broadcast_to([128, 8, 128]), op=ALU.mult),
                     reads=[ebk + "_0", ebk + "_1", "maskT"], writes=[ptk])

            def stage_b(j):
                ptj, ptk = eb[j % 2], "pT%d" % (j % 2)
                for h in range(NH):
                    par, pr = h % 2, h // 2
                    bk = h // 4
                    P.op("pe", lambda e, par=par, pr=pr, h=h, bk=bk, ptj=ptj, j=j: e.matmul(
                        psD[bk][:, (h % 4) * 65:(h % 4) * 65 + 65], lhsT=ptj[:, par, pr, :], rhs=av1[:, j, :],
                        start=(j == 0 and h % 4 == 0), stop=(j == i and h % 4 == 3), skip_group_check=True),
                         reads=[ptk, "av1_%d" % j, "av1_ones"], writes=["psD%d" % bk])

            stage_a(0)
            for j in range(i + 1):
                if j + 1 <= i:
                    stage_a(j + 1)
                stage_b(j)
                yield
            for bk in range(2):
                pv = psD[bk][:, 0:260].rearrange("p (k d) -> p k d", d=65)
                P.op("dve", lambda e, pv=pv, bk=bk: e.reciprocal(out=st8[:, 5, bk * 4:(bk + 1) * 4].unsqueeze(2),
                                                                 in_=pv[:, :, 64:65]),
                     reads=["psD%d" % bk], writes=["st8_5%d" % bk])
                P.op("dve", lambda e, pv=pv, bk=bk: e.tensor_tensor(
                    out=att1[:, bk * 256:(bk + 1) * 256].rearrange("p (k d) -> p k d", d=64),
                    in0=pv[:, :, 0:64], in1=st8[:, 5, bk * 4:(bk + 1) * 4].unsqueeze(2).broadcast_to([128, 4, HD]),
                    op=ALU.mult), reads=["psD%d" % bk, "st8_5%d" % bk], writes=["att_%d" % bk])
            yield
            for _ in outproj(i):
                yield

        def outproj(i):
            xbuf, xk = xb[i % 4], "xb%d" % (i % 4)
            ret, retk = ret3[i % 3], "ret%d" % (i % 3)
            for kc in range(KD):
                src = ret[:, kc * 128:(kc + 1) * 128] if kc < 4 else att1[:, (kc - 4) * 128:(kc - 3) * 128]
                rk_ = [retk] if kc < 4 else ["att_0", "att_1"]
                P.op("pe", lambda e, kc=kc, src=src: e.transpose(out=psA1v[:, kc * 128:(kc + 1) * 128], in_=src,
                                                                 identity=ident[:]),
                     reads=rk_ + ["ident"], writes=["psA1"])
            P.op("act", lambda e: e.copy(out=mixT[:], in_=psA1v.rearrange("p (k t) -> p k t", k=KD)),
                 reads=["psA1"], writes=["mixT"])
            yield
            ob = [(psC[0], "psC0"), (psC[1], "psC1")]
            for half in range(2):
                pb, pbk = ob[half]
                for kc in range(KD):
                    P.op("pe", lambda e, kc=kc, pb=pb, half=half: e.matmul(
                        pb[:], lhsT=mixT[:, kc, :], rhs=w_out_sb[:, kc, half * 512:(half + 1) * 512],
                        start=(kc == 0), stop=(kc == KD - 1)), reads=["mixT", "w_out%d" % half], writes=[pbk])
                P.op("act", lambda e, pb=pb, half=half: e.activation(
                    out=junk2[:, half * 512:(half + 1) * 512], in_=pb[:], func=AF.Square,
                    accum_out=sm[:, 4 + half:5 + half]), reads=[pbk], writes=["junk1", "smd%d" % half])
            P.op("pool", lambda e: e.tensor_tensor(out=sm[:, 3:4], in0=sm[:, 4:5], in1=sm[:, 5:6], op=ALU.add),
                 reads=["smd0", "smd1"], writes=["sm3"])
            rstd_from_ss(sm[:, 3:4], sm[:, 7:8], sm[:, 6:7], "sm3", "sm6", "sm7", 1, 1.0 / D)
            yield
            for half in range(2):
                pb, pbk = ob[half]
                P.op("dve", lambda e, pb=pb, half=half: e.tensor_tensor(
                    out=pb[:], in0=pb[:], in1=gpost[:, half * 512:(half + 1) * 512], op=ALU.mult),
                     reads=[pbk, "gbc"], writes=[pbk])
                P.op("dve", lambda e, pb=pb, half=half, xbuf=xbuf: e.scalar_tensor_tensor(
                    out=xbuf[:, half * 512:(half + 1) * 512], in0=pb[:], scalar=sm[:, 7:8],
                    in1=xbuf[:, half * 512:(half + 1) * 512], op0=ALU.mult, op1=ALU.add),
                     reads=[pbk, "sm7", xk], writes=[xk])
            P.op("sp", lambda e, xbuf=xbuf, i=i: e.dma_start(out=out[i * 128:(i + 1) * 128, :], in_=xbuf[:]),
                 reads=[xk], writes=["h1row%d" % i], dma="h1st%d" % (i % 4))
            yield

        def merge(gens):
            gens = [[g, 0, max(1, n)] for g, n in gens]
            live = list(gens)
            while live:
                live.sort(key=lambda t: (t[1] + 1) / t[2])
                t = live[0]
                try:
                    next(t[0])
                    t[1] += 1
                except StopIteration:
                    live.remove(t)

        def run_all(g):
            for _ in g:
                pass

        def front_units(i):
            return 13

        def front_index(i):
            for _ in front(i):
                yield
            for _ in indexmm(i):
                yield

        run_all(front_index(0))
        run_all(chain(0))
        mask_tr(0)
        if NT > 1:
            run_all(front_index(1))
        for i_ in range(NT):
            gens = [(attn(i_), i_ + 5)]
            if i_ + 1 < NT:
                gens.append((chain(i_ + 1), NB + 2))
            if i_ + 2 < NT:
                gens.append((front_index(i_ + 2), 13 + (i_ + 6) // 4))
            merge(gens)
            if i_ + 1 < NT:
                mask_tr(i_ + 1)
        if not do_ffn:
            P.final_wait("sp", ["h1st0", "h1st1", "h1st2", "h1st3"])
        P.barrier()
        P.emit()
        es1.close()

    if do_ffn:
        es2 = es
        gbc = sb("gbc2", [128, 2, D])
        P.op("sp", lambda e: e.dma_start(out=gbc[:].rearrange("p g d -> p (g d)"),
                                         in_=gains[2:4, :].rearrange("g d -> (g d)").partition_broadcast(128)),
             writes=["gbc"], dma="c4")
        w_up_sb = sb("w_up_sb", [128, KD, 2 * D_FF], BF16)
        w_down_sb = sb("w_down_sb", [128, NFC, D], BF16)
        cw = sb("cw", [128, 2 * NFC, 4])
        h1b = sb("h1b", [128, 2, D])
        hn_bf = sb("hn_bf", [128, D], BF16)
        junk = sb("junk", [128, D], BF16)
        hnT = sb("hnT", [128, KD, TS2], BF16)
        upb = [sb("upb%d" % i, [128, TS2 + 2]) for i in range(4)]
        yb = [sb("yb%d" % i, [128, TS2]) for i in range(4)]
        sgb = [sb("sgb%d" % i, [128, TS2]) for i in range(2)]
        actT = sb("actT", [128, NFC, TS2], BF16)
        carry = sb("carry", [128, 2 * NFC, 2])
        ssb = sb("ssb", [128, 8])
        tmpf = sb("tmpf", [128, D])

        P.op("sp", lambda e: e.dma_start(out=cw[:], in_=cwb), writes=["cw"], dma="c2")
        P.op("pool", lambda e: e.memset(carry[:], 0.0), writes=["carry"])
        wup_v = w_up.rearrange("(kc p) f -> p kc f", p=128)
        for c4 in range(0, NFC, 4):
            n4 = min(4, NFC - c4)
            for base in (0, NFC):
                f0, f1 = (base + c4) * 128, (base + c4 + n4) * 128
                P.op("pool", lambda e, f0=f0, f1=f1: e.dma_start(out=w_up_sb[:, :, f0:f1], in_=wup_v[:, :, f0:f1]),
                     writes=["wup%d" % fc for fc in range(base + c4, base + c4 + n4)],
                     dma="wup%d_%d" % (c4 // 4, base))
        wdn_v = w_down.rearrange("(fc p) n -> p fc n", p=128)
        for c in range(0, NFC, 2):
            P.op("pool", lambda e, c=c: e.dma_start(out=w_down_sb[:, c:c + 2, :], in_=wdn_v[:, c:c + 2, :]),
                 writes=["wdn%d" % c, "wdn%d" % (c + 1)], dma="wdn%d" % (c // 2))

        h1b2 = [h1b, sb("h1b_b", [128, 2, D])]
        hnT2 = [hnT, sb("hnT_b", [128, KD, TS2], BF16)]
        hn2 = [hn_bf, sb("hn_bf_b", [128, D], BF16)]

        def ffn_norm(st):
            h1b = h1b2[st % 2]
            for sub in range(2):
                hn_bf = hn2[sub]
                hnk = "hn_bf%d" % sub
                r0 = (st * 2 + sub) * 128
                hk = "h1b%d_%d" % (st % 2, sub)
                P.op("sp", lambda e, sub=sub, r0=r0, h1b=h1b: e.dma_start(out=h1b[:, sub, :], in_=h1_src[r0:r0 + 128, :]),
                     reads=["h1row%d" % (st * 2 + sub)], writes=[hk], dma="h1ld%d_%d" % (st % 2, sub))
                P.op("act", lambda e, sub=sub, h1b=h1b: e.activation(out=junk[:], in_=h1b[:, sub, :], func=AF.Square,
                                                            accum_out=ssb[:, 0:1]),
                     reads=[hk], writes=["junk", "ss0"])
                rstd_from_ss(ssb[:, 0:1], ssb[:, 2:3], ssb[:, 1:2], "ss0", "ss1", "ss2", 1, 1.0 / D)
                P.op("dve", lambda e, sub=sub, hn_bf=hn_bf, h1b=h1b: e.scalar_tensor_tensor(
                    out=hn_bf[:], in0=h1b[:, sub, :], scalar=ssb[:, 2:3], in1=gbc[:, 0, :],
                    op0=ALU.mult, op1=ALU.mult), reads=[hk, "ss2", "gbc"], writes=[hnk])

        def ffn_tr(st):
            hnT = hnT2[st % 2]
            hnTk = "hnT%d" % (st % 2)
            for sub in range(2):
                hn_bf = hn2[sub]
                hnk = "hn_bf%d" % sub
                pa = psAv[sub]
                pk = "psA%d" % sub
                for kc in range(KD):
                    P.op("pe", lambda e, kc=kc, pa=pa, hn_bf=hn_bf: e.transpose(out=pa[:, kc * 128:(kc + 1) * 128],
                                                                  in_=hn_bf[:, kc * 128:(kc + 1) * 128],
                                                                  identity=ident[:]),
                         reads=[hnk, "ident"], writes=[pk])
                P.op("act", lambda e, sub=sub, pa=pa, hnT=hnT: e.copy(
                    out=hnT[:, :, sub * 128:(sub + 1) * 128],
                    in_=pa.rearrange("p (k t) -> p k t", k=KD)), reads=[pk], writes=[hnTk])

        def ffn_up(st):
            hnT = hnT2[st % 2]
            hnTk = "hnT%d" % (st % 2)
            for c in range(NFC):
                ybs = []
                for gi, fc in enumerate((c, c + NFC)):
                    bi = (c % 2) * 2 + gi
                    pb = psB[gi] if c % 2 == 0 else psC[gi]
                    pbk = ("psB%d" if c % 2 == 0 else "psC%d") % gi
                    ub, ubk = upb[bi], "upb%d" % bi
                    y, yk = yb[bi], "yb%d" % bi
                    for kc in range(KD):
                        P.op("pe", lambda e, kc=kc, fc=fc, pb=pb, hnT=hnT: e.matmul(
                            pb[:, 0:TS2], lhsT=w_up_sb[:, kc, fc * 128:(fc + 1) * 128], rhs=hnT[:, kc, :],
                            start=(kc == 0), stop=(kc == KD - 1)),
                             reads=["wup%d" % fc, hnTk], writes=[pbk])
                    P.op("pool", lambda e, ub=ub, fc=fc: e.tensor_copy(out=ub[:, 0:2], in_=carry[:, fc, :]),
                         reads=["carry%d" % fc, "carry"], writes=[ubk])
                    P.op("act", lambda e, ub=ub, pb=pb: e.copy(out=ub[:, 2:TS2 + 2], in_=pb[:, 0:TS2]),
                         reads=[pbk], writes=[ubk])
                    P.op("act", lambda e, y=y, pb=pb, fc=fc: e.activation(
                        out=y[:], in_=pb[:, 0:TS2], func=AF.Identity, bias=cw[:, fc, 3:4], scale=cw[:, fc, 2:3]),
                         reads=[pbk, "cw"], writes=[yk])
                    P.op("pool", lambda e, ub=ub, fc=fc: e.tensor_copy(out=carry[:, fc, :], in_=ub[:, TS2:TS2 + 2]),
                         reads=[ubk], writes=["carry%d" % fc])
                    P.op("dve", lambda e, y=y, ub=ub, fc=fc: e.scalar_tensor_tensor(
                        out=y[:], in0=ub[:, 1:TS2 + 1], scalar=cw[:, fc, 1:2], in1=y[:], op0=ALU.mult, op1=ALU.add),
                         reads=[ubk, yk, "cw"], writes=[yk])
                    P.op("dve", lambda e, y=y, ub=ub, fc=fc: e.scalar_tensor_tensor(
                        out=y[:], in0=ub[:, 0:TS2], scalar=cw[:, fc, 0:1], in1=y[:], op0=ALU.mult, op1=ALU.add),
                         reads=[ubk, yk, "cw"], writes=[yk])
                    ybs.append((y, yk))
                sg, sgk = sgb[c % 2], "sgb%d" % (c % 2)
                P.op("act", lambda e, sg=sg, y=ybs[0][0]: e.activation(out=sg[:], in_=y[:], func=AF.Silu),
                     reads=[ybs[0][1]], writes=[sgk])
                P.op("dve", lambda e, sg=sg, y=ybs[1][0], c=c: e.tensor_tensor(
                    out=actT[:, c, :], in0=sg[:], in1=y[:], op=ALU.mult),
                     reads=[sgk, ybs[1][1]], writes=["actT%d" % c])
                if c == NFC // 2 and st + 1 < NST:
                    ffn_norm(st + 1)

        def ffn_down(st):
            h1b = h1b2[st % 2]
            for sub in range(2):
                r0 = (st * 2 + sub) * 128
                hk = "h1b%d_%d" % (st % 2, sub)
                pds = [(psD[0], "psD0"), (psD[1], "psD1")] if sub == 0 else [(psB[0], "psB0"), (psB[1], "psB1")]
                for half in range(2):
                    pd = pds[half][0]
                    for c in range(NFC):
                        P.op("pe", lambda e, c=c, pd=pd, sub=sub, half=half: e.matmul(
                            pd[:], lhsT=actT[:, c, sub * 128:(sub + 1) * 128],
                            rhs=w_down_sb[:, c, half * 512:(half + 1) * 512],
                            start=(c == 0), stop=(c == NFC - 1)),
                             reads=["actT%d" % c, "wdn%d" % c], writes=[pds[half][1]])
                for half in range(2):
                    P.op("act", lambda e, half=half, pd=pds[half][0]: e.activation(
                        out=junk[:, half * 512:(half + 1) * 512], in_=pd[:], func=AF.Square,
                        accum_out=ssb[:, 4 + half:5 + half]),
                         reads=[pds[half][1]], writes=["junk", "ssd%d" % half])
                P.op("pool", lambda e: e.tensor_tensor(out=ssb[:, 3:4], in0=ssb[:, 4:5], in1=ssb[:, 5:6], op=ALU.add),
                     reads=["ssd0", "ssd1"], writes=["ss3"])
                rstd_from_ss(ssb[:, 3:4], ssb[:, 7:8], ssb[:, 6:7], "ss3", "ss6", "ss7", 1, 1.0 / D)
                for half in range(2):
                    P.op("dve", lambda e, half=half, pd=pds[half][0]: e.scalar_tensor_tensor(
                        out=tmpf[:, half * 512:(half + 1) * 512], in0=pd[:], scalar=ssb[:, 7:8],
                        in1=gbc[:, 1, half * 512:(half + 1) * 512], op0=ALU.mult, op1=ALU.mult),
                         reads=[pds[half][1], "ss7", "gbc"], writes=["tmpf%d" % half])
                P.op("pool", lambda e, sub=sub, h1b=h1b: e.tensor_tensor(out=h1b[:, sub, :], in0=tmpf[:], in1=h1b[:, sub, :],
                                                                op=ALU.add),
                     reads=["tmpf0", "tmpf1", hk], writes=[hk])
                P.op("sp", lambda e, sub=sub, r0=r0, h1b=h1b: e.dma_start(out=out[r0:r0 + 128, :], in_=h1b[:, sub, :]),
                     reads=[hk], writes=["h1row%d" % (st * 2 + sub)], dma="ost%d_%d" % (st % 2, sub))

        ffn_norm(0)
        ffn_tr(0)
        for st in range(NST):
            ffn_up(st)
            if st + 1 < NST:
                ffn_tr(st + 1)
            ffn_down(st)
        P.final_wait("sp", ["ost0_0", "ost0_1", "ost1_0", "ost1_1"])
        P.emit()
    es.close()
    return nc


def host_consts(S):
    NT = S // 128
    pos = np.arange(S, dtype=np.float32)
    inv = (10000.0 ** (-np.arange(0, 64, 2, dtype=np.float32) / 64.0)).astype(np.float32)
    ang = pos[:, None] * inv[None, :]
    cs = np.concatenate([np.cos(ang), np.sin(ang)], axis=1).astype(np.float32)
    cs_tab = np.ascontiguousarray(cs.reshape(NT, 128, 64).transpose(1, 0, 2))
    consts = np.zeros((128, 512), np.float64)
    consts[:, 0:128] = np.eye(128)
    p = np.arange(128)
    consts[:, 128:256] = (p[None, :] >= p[:, None]).astype(np.float64)
    consts[:, 256:384] = np.where(p[None, :] <= p[:, None], 0.0, -1e30)
    g = 1.0 - 2.0 ** (-5.0 - np.arange(8))
    consts[:, 384:392] = g[None, :] ** (-(p[:, None] + 1.0)) * 0.125
    hpp = np.array([2 * (k % 4) + (k // 4) for k in range(8)])
    xi = g[None, hpp] ** (p[:, None] + 1.0)
    consts[:, 392:400] = xi * xi / 64.0
    consts[:, 400:408] = xi * 0.5
    for pr in range(4):
        consts[:, 408 + pr] = np.where(p < 64, g[2 * pr] ** 128.0, g[2 * pr + 1] ** 128.0)
    consts[:, 412:412 + N_BISECT] = 2.0 ** (-(np.arange(N_BISECT) + 1.0))[None, :]
    return cs_tab, consts.astype(np.float32)


def prep_inputs(b, S, x, mix_norm_pre, mix_norm_post, w_in, w_out, ffn_norm_pre, ffn_norm_post,
                w_up, conv_w, conv_b, w_down, shared):
    return dict(shared, x=np.ascontiguousarray(x[b, :S]))


def prep_shared(S, mix_norm_pre, mix_norm_post, w_in, w_out, ffn_norm_pre, ffn_norm_post,
                w_up, conv_w, conv_b, w_down):
    cs_tab, consts = host_consts(S)
    segs = [(0, 512), (512, 1024), (2048, 2560), (2688, 3200), (2560, 2624), (3200, 3264),
            (1024, 1536), (1536, 2048), (2624, 2688), (3264, 3272)]
    w_in_p = np.concatenate([w_in[0][:, a:b] for a, b in segs], axis=1)
    cwb = np.empty((128, 2 * NFC, 4), np.float32)
    cwb[:, :, 0:3] = conv_w[0].reshape(3, 2 * NFC, 128).transpose(2, 1, 0)
    cwb[:, :, 3] = conv_b[0].reshape(2 * NFC, 128).T
    gains = np.stack([mix_norm_pre[0], mix_norm_post[0], ffn_norm_pre[0], ffn_norm_post[0]], axis=0)
    consts = consts.copy()
    consts[:, 440:448] = mix_norm_pre[0].reshape(8, 128).T
    return dict(w_in=np.ascontiguousarray(w_in_p, dtype=np.float32), w_out=np.ascontiguousarray(w_out[0]),
                w_up=np.ascontiguousarray(w_up[0]), w_down=np.ascontiguousarray(w_down[0]),
                cwb=cwb, gains=np.ascontiguousarray(gains, dtype=np.float32), cs_tab=cs_tab, consts=consts)


def kernel(**inputs):
    inputs = {k: np.asarray(v) for k, v in inputs.items()}
    x = inputs["x"]
    B, S, _ = x.shape
    nc = build_nc(S)
    wkeys = ["mix_norm_pre", "mix_norm_post", "w_in", "w_out", "ffn_norm_pre", "ffn_norm_post",
             "w_up", "conv_w", "conv_b", "w_down"]
    shared = prep_shared(S, *[inputs[k] for k in wkeys])
    in_maps = [dict(shared, x=np.ascontiguousarray(x[b])) for b in range(B)]
    res = run_bass_kernel_spmd(nc, in_maps, core_ids=list(range(B)))
    return np.stack([np.asarray(r["out"]) for r in res.results], axis=0).astype(np.float32)
```

```python
from contextlib import ExitStack
import numpy as np
import ml_dtypes

import concourse.bass as bass
import concourse.mybir as mybir
from concourse.bass_utils import run_bass_kernel_spmd

F32 = mybir.dt.float32
BF16 = mybir.dt.bfloat16
AF = mybir.ActivationFunctionType
ALU = mybir.AluOpType
AX = mybir.AxisListType

D = 1024
KD = 8
HD = 64
NH = 8
IN_W = 3272
D_FF = 2816
NFC = 22
EPS = 1e-6
TOPK = 256
N_BISECT = 13

C_RQ, C_RK, C_AQ, C_IQ = 0, 512, 1024, 1536
C_AK, C_IK = 2048, 2112
C_RV, C_RG, C_AV, C_IW = 2176, 2688, 3200, 3264


class Prog:
    CE = ("pe", "act", "dve", "pool")

    def __init__(self, nc, es):
        self.nc = nc
        self.es = es
        self.lists = {e: [] for e in ("pe", "act", "dve", "pool", "sp")}
        self.cnt = {e: 0 for e in self.CE}
        self.psem = {e: es.enter_context(nc.semaphore("prog_" + e)) for e in self.CE}
        self.dstream = {}
        self.res = {}
        self.seen = {e: {} for e in self.lists}

    def _stream(self, name, group=False):
        if name not in self.dstream:
            self.dstream[name] = [self.es.enter_context(self.nc.semaphore("dq_" + name)), 0, group]
        return self.dstream[name]

    def op(self, eng, fn, reads=(), writes=(), dma=None, group=False):
        waits = {}
        if dma is not None:
            st = self._stream(dma, group)
            if not st[2] and st[1] > 0:
                waits[("d", dma)] = st[1]

        def need(tok, kind):
            if tok is None:
                return
            tk, who, val = tok
            if tk == "c":
                if who == eng and dma is None:
                    if eng == "pe":
                        return
                key = ("c", who)
                waits[key] = max(waits.get(key, 0), val)
            else:
                key = ("d", who)
                waits[key] = max(waits.get(key, 0), self.dstream[who][1])

        for r in reads:
            ent = self.res.get(r)
            if ent:
                need(ent[0], "raw")
        for w in writes:
            ent = self.res.get(w)
            if ent:
                need(ent[0], "waw")
                for tok in ent[1].values():
                    need(tok, "war")
        if dma is not None:
            st = self._stream(dma)
            st[1] += 16
            tok = ("d", dma, st[1])
            inc = (st[0], 16)
        else:
            self.cnt[eng] += 1
            tok = ("c", eng, self.cnt[eng])
            inc = (self.psem[eng], 1)
        for r in reads:
            ent = self.res.setdefault(r, [None, {}])
            ent[1][(tok[0], tok[1])] = tok
        for w in writes:
            self.res[w] = [tok, {}]
        wl = []
        seen = self.seen[eng]
        for key, val in waits.items():
            if seen.get(key, 0) >= val:
                continue
            seen[key] = val
            wl.append((key, val))
        self.lists[eng].append((fn, wl, inc))

    def barrier(self):
        for eng in self.lists:
            wl = [(("c", e), self.cnt[e]) for e in self.CE if e != eng and self.cnt[e] > 0]
            wl += [(("d", s), st[1]) for s, st in self.dstream.items() if st[1] > 0]
            self.lists[eng].append((None, wl, None))
            for key, val in wl:
                self.seen[eng][key] = max(self.seen[eng].get(key, 0), val)

    def final_wait(self, eng, streams):
        wl = [(("d", s), self.dstream[s][1]) for s in streams if s in self.dstream]
        self.lists[eng].append((None, wl, None))

    def emit(self):
        nc = self.nc
        lists = self.lists

        def run(e, items):
            for fn, wl, inc in items:
                for key, val in wl:
                    if key[0] == "c":
                        sem = self.psem[key[1]]
                    else:
                        st = self.dstream[key[1]]
                        sem = st[0]
                        if st[2]:
                            val = st[1]
                    e.wait_ge(sem, val)
                if fn is not None:
                    ins = fn(e)
                    ins.then_inc(inc[0], inc[1])

        self.lists = {e: [] for e in lists}
        with nc.Block() as block:
            @block.tensor
            def _(e):
                run(e, lists["pe"])

            @block.scalar
            def _(e):
                run(e, lists["act"])

            @block.vector
            def _(e):
                run(e, lists["dve"])

            @block.gpsimd
            def _(e):
                run(e, lists["pool"])

            @block.sync
            def _(e):
                run(e, lists["sp"])


def build_nc(S, do_mixer=True, do_ffn=True, dbg=False):
    NT = S // 128
    TS2 = 256
    NST = S // TS2
    nc = bass.Bass("TRN2", target_bir_lowering=False)
    es = ExitStack()

    def din(name, shape, dt=F32):
        return nc.dram_tensor(name, list(shape), dt, kind="ExternalInput").ap()

    x = din("x", [S, D])
    w_in = din("w_in", [D, IN_W])
    w_out = din("w_out", [D, D])
    w_up = din("w_up", [D, 2 * D_FF])
    w_down = din("w_down", [D_FF, D])
    cwb = din("cwb", [128, 2 * NFC, 4])
    gains = din("gains", [4, D])
    cs_tab = din("cs_tab", [128, NT, 64])
    consts = din("consts", [128, 512])
    out = nc.dram_tensor("out", [S, D], F32, kind="ExternalOutput").ap()

    P = Prog(nc, es)

    def sb(name, shape, dt=F32):
        return es.enter_context(nc.sbuf_tensor(name, list(shape), dt))

    def ps(name, shape, dt=F32):
        return es.enter_context(nc.psum_tensor(name, list(shape), dt))

    ident = sb("ident", [128, 128], BF16)
    cst = sb("cst", [128, 512])
    neg_half = sb("neg_half", [128, 8])
    P.op("sp", lambda e: e.dma_start(out=cst[:], in_=consts), writes=["cst"], dma="c0")
    P.op("dve", lambda e: e.tensor_copy(out=ident[:], in_=cst[:, 0:128]), reads=["cst"], writes=["ident"])
    P.op("pool", lambda e: e.memset(neg_half[:], -0.5), writes=["neg_half"])

    psA = [ps("psA0", [128, 1024], BF16), ps("psA1", [128, 512])]
    psA1v = psA[1][:].bitcast(BF16)
    psAv = [psA[0][:], psA1v]
    psB = [ps("psB%d" % i, [128, 512]) for i in range(2)]
    psC = [ps("psC%d" % i, [128, 512]) for i in range(2)]
    psD = [ps("psD%d" % i, [128, 512]) for i in range(2)]

    def rstd_from_ss(ss_ap, rstd_ap, tmp_ap, key_ss, key_tmp, key_rstd, n, inv_n):
        P.op("pool", lambda e: e.tensor_scalar(out=tmp_ap, in0=ss_ap, scalar1=inv_n, scalar2=EPS,
                                               op0=ALU.mult, op1=ALU.add),
             reads=[key_ss], writes=[key_tmp])
        P.op("pool", lambda e: e.tensor_tensor(out=rstd_ap, in0=tmp_ap, in1=neg_half[:, 0:n], op=ALU.pow),
             reads=[key_tmp, "neg_half"], writes=[key_rstd])

    h1_src = out if do_mixer else x
    TOPK_ = min(TOPK, S // 4)
    NB = N_BISECT

    if do_mixer:
        es1 = ExitStack()

        def sb1(name, shape, dt=F32):
            return es1.enter_context(nc.sbuf_tensor(name, list(shape), dt))

        dbg_mix = None
        if dbg:
            dbg_mix = nc.dram_tensor("dbg_mix", [S, D], BF16, kind="ExternalOutput").ap()
        gpost = sb1("gpost", [128, D])
        P.op("sp", lambda e: e.dma_start(out=gpost[:], in_=gains[1, :].partition_broadcast(128)),
             writes=["gbc"], dma="c1")

        w_in_sb = sb1("w_in_sb", [128, KD, IN_W], BF16)
        w_out_sb = sb1("w_out_sb", [128, KD, D], BF16)
        cs2 = [sb1("cs%d" % k, [128, 64]) for k in range(2)]
        xb = [sb1("xb%d" % k, [128, D]) for k in range(4)]
        xn_bf = sb1("xn_bf", [128, D], BF16)
        junk1 = sb1("junk1", [128, D], BF16)
        junk2 = junk1
        xnT = sb1("xnT", [128, KD, 128], BF16)
        qk_tok = sb1("qk_tok", [128, 2304], BF16)
        rt = sb1("rt", [128, 1024])
        vtok = sb1("vtok", [128, NH, HD], BF16)
        g_s = sb1("g_s", [128, 512])
        g_e = sb1("g_e", [128, 512])
        g_sg = g_e
        rqkT = sb1("rqkT", [128, 8, 128], BF16)
        aqT3 = [sb1("aqT%d" % k, [128, 4, 128], BF16) for k in range(3)]
        iqTz = sb1("iqTz", [128, NH, 128], BF16)
        akT2 = sb1("akT2", [128, S], BF16)
        ikT2 = sb1("ikT2", [128, S], BF16)
        av1 = sb1("av1", [128, NT, 65], BF16)
        iwb = sb1("iwb", [128, 8])
        scT = sb1("scT", [128, 2, 4, 128], BF16)
        R32 = sb1("R32", [128, 4, HD])
        Rbf = sb1("Rbf", [128, 4, HD], BF16)
        osq = sb1("osq", [128, 2, 4, HD])
        st8 = sb1("st8", [128, 8, 8])
        iscore2 = [sb1("iscore%d" % k, [128, S]) for k in range(2)]
        relb = [sb1("relb%d" % k, [128, 512], BF16) for k in range(4)]
        diagw = sb1("diagw", [128, NH, 128], BF16)
        maskb_ = sb1("maskb", [128, S], BF16)
        maskb2 = [maskb_, maskb_]
        maskT = sb1("maskT", [128, NT, 128], BF16)
        eb = [sb1("eb%d" % k, [128, 2, 4, 128], BF16) for k in range(2)]
        ret3 = [sb1("ret%d" % k, [128, 512], BF16) for k in range(3)]
        att1 = sb1("att1", [128, 512], BF16)
        mixT = sb1("mixT", [128, KD, 128], BF16)
        bs = sb1("bs", [128, 8 + NB])
        sm = sb1("sm", [128, 16])

        maskT01 = cst[:, 128:256]
        negmask = cst[:, 256:384]
        kscale = cst[:, 384:392]
        xi2_64 = cst[:, 392:400]
        xi_pp = cst[:, 400:408]
        gC = cst[:, 408:412]
        pow2 = cst[:, 412:412 + NB]

        P.op("sp", lambda e: e.dma_start(out=cs2[0][:], in_=cs_tab[:, 0, :]), writes=["cs0"], dma="csld0")
        win_v = w_in.rearrange("(kc p) f -> p kc f", p=128)
        bounds = [0, 512, 1024, 1536, 2048, 2176, 2688, 3200, IN_W]
        for k in range(8):
            a, b = bounds[k], bounds[k + 1]
            P.op("pool", lambda e, a=a, b=b: e.dma_start(out=w_in_sb[:, :, a:b], in_=win_v[:, :, a:b]),
                 writes=["w_in%d" % k], dma="win%d" % k)
            P.op("dve", lambda e, a=a, b=b: e.tensor_tensor(
                out=w_in_sb[:, :, a:b], in0=w_in_sb[:, :, a:b],
                in1=cst[:, 440:448].unsqueeze(2).broadcast_to([128, KD, b - a]), op=ALU.mult),
                 reads=["w_in%d" % k, "cst"], writes=["w_in%d" % k])
        wout_v = w_out.rearrange("(kc p) f -> p kc f", p=128)
        for k in range(2):
            P.op("pool", lambda e, k=k: e.dma_start(out=w_out_sb[:, :, k * 512:(k + 1) * 512],
                                                    in_=wout_v[:, :, k * 512:(k + 1) * 512]),
                 writes=["w_out%d" % k], dma="wout", group=True)
        P.op("pool", lambda e: e.memset(av1[:, :, 64:65], 1.0), writes=["av1_ones"])
        P.op("pool", lambda e: e.memset(R32[:], 0.0), writes=["R32"])
        P.op("pool", lambda e: e.memset(iqTz[:], 0.0), writes=["iqT"])
        P.op("sp", lambda e: e.dma_start(out=xb[0][:], in_=x[0:128, :]), writes=["xb0"], dma="xld0")

        frot = [0]

        def next_bank():
            b = (psB[frot[0] % 2], "psB%d" % (frot[0] % 2))
            frot[0] += 1
            return b

        def front(i):
            xbuf, xk = xb[i % 4], "xb%d" % (i % 4)
            aqT, aqk = aqT3[i % 3], "aqT%d" % (i % 3)
            ret, retk = ret3[i % 3], "ret%d" % (i % 3)
            cs, csk = cs2[i % 2], "cs%d" % (i % 2)
            if i + 1 < NT:
                P.op("sp", lambda e, i=i: e.dma_start(out=cs2[(i + 1) % 2][:], in_=cs_tab[:, i + 1, :]),
                     writes=["cs%d" % ((i + 1) % 2)], dma="csld%d" % ((i + 1) % 2))
                nb_, nk_ = xb[(i + 1) % 4], "xb%d" % ((i + 1) % 4)
                P.op("sp", lambda e, nb_=nb_, i=i: e.dma_start(out=nb_[:], in_=x[(i + 1) * 128:(i + 2) * 128, :]),
                     writes=[nk_], dma="xld%d" % ((i + 1) % 4))
            P.op("act", lambda e, xbuf=xbuf: e.activation(out=junk1[:], in_=xbuf[:], func=AF.Square,
                                                          accum_out=sm[:, 0:1]),
                 reads=[xk], writes=["junk1", "sm0"])
            rstd_from_ss(sm[:, 0:1], sm[:, 2:3], sm[:, 1:2], "sm0", "sm1", "sm2", 1, 1.0 / D)
            P.op("dve", lambda e, xbuf=xbuf: e.tensor_scalar(
                out=xn_bf[:], in0=xbuf[:], scalar1=sm[:, 2:3], scalar2=None, op0=ALU.mult),
                 reads=[xk, "sm2"], writes=["xn_bf"])
            for kc in range(KD):
                P.op("pe", lambda e, kc=kc: e.transpose(out=psA[0][:, kc * 128:(kc + 1) * 128],
                                                        in_=xn_bf[:, kc * 128:(kc + 1) * 128], identity=ident[:]),
                     reads=["xn_bf", "ident"], writes=["psA0"])
            P.op("act", lambda e: e.copy(out=xnT[:], in_=psA[0][:].rearrange("p (k t) -> p k t", k=KD)),
                 reads=["psA0"], writes=["xnT"])
            yield

            def proj_slice(k):
                a, b = bounds[k], bounds[k + 1]
                pb, pbk = next_bank()
                for kc in range(KD):
                    P.op("pe", lambda e, kc=kc, pb=pb, a=a, b=b: e.matmul(
                        pb[:, 0:b - a], lhsT=xnT[:, kc, :], rhs=w_in_sb[:, kc, a:b],
                        start=(kc == 0), stop=(kc == KD - 1)),
                         reads=["xnT", "w_in%d" % k], writes=[pbk])
                return pb, pbk

            def rope(pb, pbk, nh, dst3, dkey):
                P.op("act", lambda e: e.copy(out=rt[:, 0:nh * 64], in_=pb[:, 0:nh * 64]), reads=[pbk], writes=["rt_x"])
                src = rt[:, 0:nh * 64].rearrange("p (h two f) -> p h two f", two=2, f=32)
                x1, x2 = src[:, :, 0, :], src[:, :, 1, :]
                c = cs[:, 0:32].unsqueeze(1).broadcast_to([128, nh, 32])
                sn = cs[:, 32:64].unsqueeze(1).broadcast_to([128, nh, 32])
                ta = rt[:, 512:512 + nh * 32].rearrange("p (h f) -> p h f", f=32)
                tb = rt[:, 768:768 + nh * 32].rearrange("p (h f) -> p h f", f=32)
                for (xa, xb_, op_, dst_) in ((x1, x2, ALU.subtract, dst3[:, :, 0:32]), (x2, x1, ALU.add, dst3[:, :, 32:64])):
                    P.op("pool", lambda e, xa=xa: e.tensor_tensor(out=ta, in0=xa, in1=c, op=ALU.mult),
                         reads=["rt_x", csk], writes=["rt_a"])
                    P.op("pool", lambda e, xb_=xb_: e.tensor_tensor(out=tb, in0=xb_, in1=sn, op=ALU.mult),
                         reads=["rt_x", csk], writes=["rt_b"])
                    P.op("pool", lambda e, op_=op_, dst_=dst_: e.tensor_tensor(out=dst_, in0=ta, in1=tb, op=op_),
                         reads=["rt_a", "rt_b"], writes=[dkey])

            for k in range(4):
                pb, pbk = proj_slice(k)
                rope(pb, pbk, 8, qk_tok[:, k * 512:(k + 1) * 512].rearrange("p (h d) -> p h d", d=64), "qk_tok%d" % k)
                yield
            pb, pbk = proj_slice(4)
            kk4 = qk_tok[:, 2048:2304].rearrange("p (h r d) -> p h r d", h=2, r=2)
            rope(pb, pbk, 2, kk4[:, :, 0, :], "qk_tok4")
            P.op("pool", lambda e: e.tensor_copy(out=kk4[:, :, 1, :], in_=kk4[:, :, 0, :]),
                 reads=["qk_tok4"], writes=["qk_tok4b"])
            yield
            pb, pbk = proj_slice(5)
            P.op("act", lambda e, pb=pb: e.copy(out=rt[:, 0:512], in_=pb[:]), reads=[pbk], writes=["rt_x"])
            P.op("pool", lambda e: e.tensor_tensor(
                out=vtok[:], in0=rt[:, 0:512].rearrange("p (h d) -> p h d", d=64),
                in1=kscale.unsqueeze(2).broadcast_to([128, NH, HD]), op=ALU.mult),
                 reads=["rt_x", "cst"], writes=["vtok"])
            yield
            pb, pbk = proj_slice(6)
            P.op("act", lambda e, pb=pb: e.activation(out=g_e[:], in_=pb[:], func=AF.Tanh, scale=0.5),
                 reads=[pbk], writes=["g_e"])
            P.op("act", lambda e, pb=pb: e.copy(out=g_s[:], in_=pb[:]), reads=[pbk], writes=["g_s"])
            P.op("pool", lambda e: e.tensor_scalar(out=g_e[:], in0=g_e[:], scalar1=1.0, scalar2=1.0, op0=ALU.add,
                                                   op1=ALU.mult), reads=["g_e"], writes=["g_e"])
            P.op("pool", lambda e: e.tensor_tensor(out=g_sg[:], in0=g_s[:], in1=g_e[:], op=ALU.mult),
                 reads=["g_s", "g_e"], writes=["g_e", "g_sg"])
            yield
            pb, pbk = proj_slice(7)
            P.op("act", lambda e, pb=pb: e.copy(out=av1[:, i, 0:64], in_=pb[:, 0:64]), reads=[pbk], writes=["av1_%d" % i])
            P.op("act", lambda e, pb=pb: e.mul(out=iwb[:], in_=pb[:, 64:72], mul=float((8 ** -0.5) * (64 ** -0.5))),
                 reads=[pbk], writes=["iwb"])

            P.op("pool", lambda e: e.tensor_tensor(
                out=diagw[:], in0=ident[:].unsqueeze(1).broadcast_to([128, NH, 128]),
                in1=iwb[:].unsqueeze(2).broadcast_to([128, NH, 128]), op=ALU.mult),
                 reads=["ident", "iwb"], writes=["diagw"])
            yield
            for rnd in range(2):
                for bl in range(8):
                    gb = rnd * 8 + bl
                    P.op("pe", lambda e, bl=bl, gb=gb: e.transpose(
                        out=psA[0][:, bl * 128:(bl + 1) * 128], in_=qk_tok[:, gb * 128:(gb + 1) * 128], identity=ident[:]),
                         reads=["qk_tok%d" % (gb // 4), "ident"], writes=["psA0"])
                if rnd == 0:
                    P.op("act", lambda e: e.copy(out=rqkT[:], in_=psA[0][:].rearrange("p (k t) -> p k t", k=8)),
                         reads=["psA0"], writes=["rqkT"])
                else:
                    P.op("act", lambda e: e.copy(out=aqT[:], in_=psA[0][:, 0:512].rearrange("p (k t) -> p k t", k=4)),
                         reads=["psA0"], writes=[aqk])
                    for par in range(2):
                        P.op("act", lambda e, par=par: e.copy(
                            out=iqTz[par * 64:(par + 1) * 64].rearrange("p (b two) t -> p b two t", two=2)[:, :, par, :],
                            in_=psA[0][par * 64:(par + 1) * 64, 512:1024].rearrange("p (k t) -> p k t", k=4)),
                             reads=["psA0"], writes=["iqT"])
                yield
            for bl in range(2):
                P.op("pe", lambda e, bl=bl: e.transpose(
                    out=psA[0][:, bl * 128:(bl + 1) * 128], in_=qk_tok[:, 2048 + bl * 128:2048 + (bl + 1) * 128],
                    identity=ident[:]), reads=["qk_tok4", "qk_tok4b", "ident"], writes=["psA0"])
            P.op("act", lambda e: e.copy(out=akT2[:, i * 128:(i + 1) * 128], in_=psA[0][:, 0:128]),
                 reads=["psA0"], writes=["akT2_%d" % i])
            P.op("act", lambda e: e.copy(out=ikT2[:, i * 128:(i + 1) * 128], in_=psA[0][:, 128:256]),
                 reads=["psA0"], writes=["ikT2_%d" % i])
            yield

            scb = [(psB[0], "psB0"), (psB[1], "psB1")]
            for h in range(NH):
                par, pr = h % 2, h // 2
                pb, pbk = scb[par]
                P.op("pe", lambda e, par=par, pr=pr, pb=pb: e.matmul(
                    pb[:, pr * 128:(pr + 1) * 128], lhsT=rqkT[par * 64:(par + 1) * 64, 4 + pr, :],
                    rhs=rqkT[par * 64:(par + 1) * 64, pr, :], start=True, stop=True),
                     reads=["rqkT"], writes=[pbk])
            for par in range(2):
                pb, pbk = scb[par]
                P.op("dve", lambda e, par=par, pb=pb: e.tensor_tensor(
                    out=scT[:, par], in0=pb[:].rearrange("p (k t) -> p k t", k=4),
                    in1=maskT01.unsqueeze(1).broadcast_to([128, 4, 128]), op=ALU.mult),
                     reads=[pbk, "cst"], writes=["scT%d" % par])
            yield
            for h in range(NH):
                par, pr = h % 2, h // 2
                P.op("pe", lambda e, par=par, pr=pr, h=h: e.matmul(
                    psB[par][:, pr * 64:(pr + 1) * 64], lhsT=scT[:, par, pr, :], rhs=vtok[:, h, :],
                    start=(pr == 0), stop=(i == 0 and pr == 3), skip_group_check=True),
                     reads=["scT%d" % par, "vtok"], writes=["psB%d" % par])
            if i > 0:
                for h in range(NH):
                    par, pr = h % 2, h // 2
                    P.op("pe", lambda e, par=par, pr=pr: e.matmul(
                        psB[par][:, pr * 64:(pr + 1) * 64], lhsT=rqkT[par * 64:(par + 1) * 64, pr, :],
                        rhs=Rbf[par * 64:(par + 1) * 64, pr, :], start=False, stop=(pr == 3),
                        skip_group_check=True),
                         reads=["rqkT", "Rbf"], writes=["psB%d" % par])
            for par in range(2):
                P.op("act", lambda e, par=par: e.activation(
                    out=osq[:, par], in_=psB[par][:, 0:256].rearrange("p (k d) -> p k d", d=64), func=AF.Square),
                     reads=["psB%d" % par], writes=["osq%d" % par])
            P.op("dve", lambda e: e.tensor_reduce(out=st8[:, 0, :], in_=osq[:].rearrange("p a k d -> p (a k) d"),
                                                  axis=AX.X, op=ALU.add),
                 reads=["osq0", "osq1"], writes=["st8_0"])
            P.op("pool", lambda e: e.tensor_tensor(out=st8[:, 1, :], in0=st8[:, 0, :], in1=xi2_64, op=ALU.mult),
                 reads=["st8_0", "cst"], writes=["st8_1"])
            rstd_from_ss(st8[:, 1, :], st8[:, 3, :], st8[:, 2, :], "st8_1", "st8_2", "st8_3", 8, 1.0)
            P.op("pool", lambda e: e.tensor_tensor(out=st8[:, 4, :], in0=st8[:, 3, :], in1=xi_pp, op=ALU.mult),
                 reads=["st8_3", "cst"], writes=["st8_4"])
            for par in range(2):
                P.op("dve", lambda e, par=par: e.tensor_tensor(
                    out=osq[:, par], in0=psB[par][:, 0:256].rearrange("p (k d) -> p k d", d=64),
                    in1=st8[:, 4, par * 4:(par + 1) * 4].unsqueeze(2).broadcast_to([128, 4, HD]), op=ALU.mult),
                     reads=["psB%d" % par, "st8_4", "osq%d" % par], writes=["osq%d" % par])
            P.op("pool", lambda e: e.tensor_tensor(
                out=ret[:].rearrange("p (k a d) -> p a k d", a=2, d=64), in0=osq[:],
                in1=g_sg[:].rearrange("p (k a d) -> p a k d", a=2, d=64), op=ALU.mult),
                 reads=["osq0", "osq1", "g_sg"], writes=[retk])
            yield
            pbS, pbSk = next_bank()
            for h in range(NH):
                par, pr = h % 2, h // 2
                P.op("pe", lambda e, par=par, pr=pr, h=h, pbS=pbS: e.matmul(
                    pbS[par * 64:(par + 1) * 64, pr * 64:(pr + 1) * 64],
                    lhsT=qk_tok[:, 512 + h * 64:512 + (h + 1) * 64], rhs=vtok[:, h, :], start=True, stop=True),
                     reads=["qk_tok1", "vtok"], writes=[pbSk])
            P.op("act", lambda e, pbS=pbS: e.copy(out=rt[:, 0:256], in_=pbS[:, 0:256]), reads=[pbSk], writes=["rt_x"])
            P.op("pool", lambda e: e.tensor_tensor(
                out=R32[:], in0=R32[:], in1=rt[:, 0:256].rearrange("p (k d) -> p k d", d=64), op=ALU.add),
                 reads=["rt_x", "R32"], writes=["R32"])
            P.op("pool", lambda e: e.tensor_tensor(out=R32[:], in0=R32[:],
                                                   in1=gC.unsqueeze(2).broadcast_to([128, 4, HD]), op=ALU.mult),
                 reads=["R32", "cst"], writes=["R32"])
            P.op("act", lambda e: e.copy(out=Rbf[:], in_=R32[:]), reads=["R32"], writes=["Rbf"])

        def indexmm(i):
            nk = (i + 1) * 128
            iscore, isk = iscore2[i % 2], "iscore%d" % (i % 2)
            ibanks = [(psB[0][:], "psB0"), (psB[1][:], "psB1"), (psA[0][:].bitcast(F32), "psA0")]
            LA = 2
            for kg in range((nk + 511) // 512):
                c0 = kg * 512
                w = min(512, nk - c0)
                acc, acck = psA[1], "psA1"

                def mm_relu(h):
                    par, pr = h % 2, h // 2
                    pb, pbk = ibanks[h % 3]
                    rb, rbk = relb[h % 4], "relb%d" % (h % 4)
                    P.op("pe", lambda e, h=h, pb=pb, c0=c0, w=w: e.matmul(
                        pb[:, 0:w], lhsT=iqTz[:, h, :], rhs=ikT2[:, c0:c0 + w], start=True, stop=True),
                         reads=["iqT"] + ["ikT2_%d" % t for t in range(c0 // 128, (c0 + w) // 128)], writes=[pbk])
                    P.op("act", lambda e, rb=rb, pb=pb, w=w: e.activation(out=rb[:, 0:w], in_=pb[:, 0:w], func=AF.Relu),
                         reads=[pbk], writes=[rbk])

                def diag(h):
                    rb, rbk = relb[h % 4], "relb%d" % (h % 4)
                    P.op("pe", lambda e, rb=rb, h=h, w=w: e.matmul(
                        psA[1][:, 0:w], lhsT=diagw[:, h, :], rhs=rb[:, 0:w], start=(h == 0), stop=(h == NH - 1)),
                         reads=[rbk, "diagw"], writes=[acck])

                for h in range(LA):
                    mm_relu(h)
                for h in range(NH):
                    diag(h)
                    if h + LA < NH:
                        mm_relu(h + LA)
                P.op("act", lambda e, c0=c0, w=w, iscore=iscore: e.copy(out=iscore[:, c0:c0 + w], in_=psA[1][:, 0:w]),
                     reads=[acck], writes=[isk])
                yield
        def chain(i):
            maskb, mbk = maskb2[i % 2], "maskb"
            iscore, isk = iscore2[i % 2], "iscore%d" % (i % 2)
            nk = (i + 1) * 128
            P.op("dve", lambda e, nk=nk: e.tensor_tensor(out=iscore[:, nk - 128:nk], in0=iscore[:, nk - 128:nk],
                                                         in1=negmask, op=ALU.add),
                 reads=[isk, "cst"], writes=[isk])
            yield
            if i * 128 >= TOPK_:
                P.op("dve", lambda e: e.tensor_reduce(out=bs[:, 0:1], in_=iscore[:, 0:TOPK_], axis=AX.X,
                                                      op=ALU.min), reads=[isk], writes=["bs0"])
                P.op("dve", lambda e, nk=nk: e.tensor_reduce(out=bs[:, 1:2], in_=iscore[:, 0:nk], axis=AX.X,
                                                             op=ALU.max), reads=[isk], writes=["bs1"])
                P.op("dve", lambda e: e.tensor_tensor(out=bs[:, 2:3], in0=bs[:, 1:2], in1=bs[:, 0:1], op=ALU.subtract),
                     reads=["bs0", "bs1"], writes=["bs2"])
                P.op("dve", lambda e: e.tensor_scalar(out=bs[:, 8:8 + NB], in0=pow2, scalar1=bs[:, 2:3], scalar2=None,
                                                      op0=ALU.mult), reads=["bs2", "cst"], writes=["bswk"])
                P.op("dve", lambda e: e.tensor_tensor(out=bs[:, 3:4], in0=bs[:, 0:1], in1=bs[:, 8:9], op=ALU.add),
                     reads=["bs0", "bswk"], writes=["bs3"])
                for k in range(NB):
                    P.op("dve", lambda e, nk=nk: e.tensor_scalar(
                        out=maskb[:, 0:nk], in0=iscore[:, 0:nk], scalar1=bs[:, 3:4], scalar2=None,
                        op0=ALU.is_ge, op1=ALU.add, accum_out=bs[:, 4:5]),
                         reads=[isk, "bs3"], writes=[mbk, "bs4"])
                    P.op("dve", lambda e: e.tensor_scalar(out=bs[:, 5:6], in0=bs[:, 4:5], scalar1=float(TOPK_) - 0.5,
                                                          scalar2=-0.5, op0=ALU.is_gt, op1=ALU.add),
                         reads=["bs4"], writes=["bs5"])
                    P.op("dve", lambda e, k=k: e.scalar_tensor_tensor(
                        out=bs[:, 3:4], in0=bs[:, 5:6], scalar=bs[:, 8 + k:9 + k], in1=bs[:, 3:4],
                        op0=ALU.mult, op1=ALU.add), reads=["bs5", "bswk", "bs3"], writes=["bs3"])
                    yield
                P.op("dve", lambda e: e.scalar_tensor_tensor(
                    out=bs[:, 6:7], in0=bs[:, 8 + NB - 1:8 + NB], scalar=-0.5, in1=bs[:, 3:4],
                    op0=ALU.mult, op1=ALU.add), reads=["bswk", "bs3"], writes=["bs6"])
                P.op("dve", lambda e, nk=nk: e.tensor_scalar(
                    out=maskb[:, 0:nk], in0=iscore[:, 0:nk], scalar1=bs[:, 6:7], scalar2=None, op0=ALU.is_ge),
                     reads=[isk, "bs6"], writes=[mbk])
            else:
                P.op("dve", lambda e, nk=nk: e.tensor_scalar(
                    out=maskb[:, 0:nk], in0=iscore[:, 0:nk], scalar1=-1e29, scalar2=None, op0=ALU.is_ge),
                     reads=[isk], writes=[mbk])

        def mask_tr(i):
            maskb, mbk = maskb2[i % 2], "maskb"
            for j0 in range(0, i + 1, 8):
                nb8 = min(8, i + 1 - j0)
                pa, pak = (psA1v, "psA1") if (j0 // 8) % 2 == 0 else (psA[0][:], "psA0")
                for bl in range(nb8):
                    j = j0 + bl
                    P.op("pe", lambda e, bl=bl, j=j, pa=pa: e.transpose(
                        out=pa[:, bl * 128:(bl + 1) * 128], in_=maskb[:, j * 128:(j + 1) * 128], identity=ident[:]),
                         reads=[mbk, "ident"], writes=[pak])
                P.op("act", lambda e, j0=j0, nb8=nb8, pa=pa: e.copy(
                    out=maskT[:, j0:j0 + nb8, :], in_=pa[:, 0:nb8 * 128].rearrange("p (k t) -> p k t", t=128)),
                     reads=[pak], writes=["maskT"])

        def attn(i):
            aqT, aqk = aqT3[i % 3], "aqT%d" % (i % 3)
            lb = [(psC[0], "psC0"), (psC[1], "psC1")]

            def stage_a(j):
                ebj, ebk = eb[j % 2], "eb%d" % (j % 2)
                ptj, ptk = eb[j % 2], "pT%d" % (j % 2)
                for par in range(2):
                    pb, pbk = lb[par]
                    P.op("pe", lambda e, par=par, pb=pb, j=j: e.matmul(
                        pb[:], lhsT=akT2[par * 64:(par + 1) * 64, j * 128:(j + 1) * 128],
                        rhs=aqT[par * 64:(par + 1) * 64, :, :], start=True, stop=True),
                         reads=[aqk, "akT2_%d" % j], writes=[pbk])
                    P.op("act", lambda e, par=par, pb=pb, ebj=ebj: e.activation(
                        out=ebj[:, par], in_=pb[:].rearrange("p (k t) -> p k t", t=128), func=AF.Exp, scale=0.125),
                         reads=[pbk], writes=[ebk + "_%d" % par])
                P.op("pool", lambda e, ebj=ebj, ptj=ptj, j=j: e.tensor_tensor(
                    out=ptj[:].rearrange("p a k t -> p (a k) t"), in0=ebj[:].rearrange("p a k t -> p (a k) t"),
                    in1=maskT[:, j, :].unsqueeze(1).broadcast_to([128, 8, 128]), op=ALU.mult),
                     reads=[ebk + "_0", ebk + "_1", "maskT"], writes=[ptk])

            def stage_b(j):
                ptj, ptk = eb[j % 2], "pT%d" % (j % 2)
                for h in range(NH):
                    par, pr = h % 2, h // 2
                    bk = h // 4
                    P.op("pe", lambda e, par=par, pr=pr, h=h, bk=bk, ptj=ptj, j=j: e.matmul(
                        psD[bk][:, (h % 4) * 65:(h % 4) * 65 + 65], lhsT=ptj[:, par, pr, :], rhs=av1[:, j, :],
                        start=(j == 0 and h % 4 == 0), stop=(j == i and h % 4 == 3), skip_group_check=True),
                         reads=[ptk, "av1_%d" % j, "av1_ones"], writes=["psD%d" % bk])

            stage_a(0)
            for j in range(i + 1):
                if j + 1 <= i:
                    stage_a(j + 1)
                stage_b(j)
                yield
            for bk in range(2):
                pv = psD[bk][:, 0:260].rearrange("p (k d) -> p k d", d=65)
                P.op("dve", lambda e, pv=pv, bk=bk: e.reciprocal(out=st8[:, 5, bk * 4:(bk + 1) * 4].unsqueeze(2),
                                                                 in_=pv[:, :, 64:65]),
                     reads=["psD%d" % bk], writes=["st8_5%d" % bk])
                P.op("dve", lambda e, pv=pv, bk=bk: e.tensor_tensor(
                    out=att1[:, bk * 256:(bk + 1) * 256].rearrange("p (k d) -> p k d", d=64),
                    in0=pv[:, :, 0:64], in1=st8[:, 5, bk * 4:(bk + 1) * 4].unsqueeze(2).broadcast_to([128, 4, HD]),
                    op=ALU.mult), reads=["psD%d" % bk, "st8_5%d" % bk], writes=["att_%d" % bk])
            yield
            for _ in outproj(i):
                yield

        def outproj(i):
            xbuf, xk = xb[i % 4], "xb%d" % (i % 4)
            ret, retk = ret3[i % 3], "ret%d" % (i % 3)
            for kc in range(KD):
                src = ret[:, kc * 128:(kc + 1) * 128] if kc < 4 else att1[:, (kc - 4) * 128:(kc - 3) * 128]
                rk_ = [retk] if kc < 4 else ["att_0", "att_1"]
                P.op("pe", lambda e, kc=kc, src=src: e.transpose(out=psA1v[:, kc * 128:(kc + 1) * 128], in_=src,
                                                                 identity=ident[:]),
                     reads=rk_ + ["ident"], writes=["psA1"])
            P.op("act", lambda e: e.copy(out=mixT[:], in_=psA1v.rearrange("p (k t) -> p k t", k=KD)),
                 reads=["psA1"], writes=["mixT"])
            yield
            ob = [(psC[0], "psC0"), (psC[1], "psC1")]
            for half in range(2):
                pb, pbk = ob[half]
                for kc in range(KD):
                    P.op("pe", lambda e, kc=kc, pb=pb, half=half: e.matmul(
                        pb[:], lhsT=mixT[:, kc, :], rhs=w_out_sb[:, kc, half * 512:(half + 1) * 512],
                        start=(kc == 0), stop=(kc == KD - 1)), reads=["mixT", "w_out%d" % half], writes=[pbk])
                P.op("act", lambda e, pb=pb, half=half: e.activation(
                    out=junk2[:, half * 512:(half + 1) * 512], in_=pb[:], func=AF.Square,
                    accum_out=sm[:, 4 + half:5 + half]), reads=[pbk], writes=["junk1", "smd%d" % half])
            P.op("pool", lambda e: e.tensor_tensor(out=sm[:, 3:4], in0=sm[:, 4:5], in1=sm[:, 5:6], op=ALU.add),
                 reads=["smd0", "smd1"], writes=["sm3"])
            rstd_from_ss(sm[:, 3:4], sm[:, 7:8], sm[:, 6:7], "sm3", "sm6", "sm7", 1, 1.0 / D)
            yield
            for half in range(2):
                pb, pbk = ob[half]
                P.op("dve", lambda e, pb=pb, half=half: e.tensor_tensor(
                    out=pb[:], in0=pb[:], in1=gpost[:, half * 512:(half + 1) * 512], op=ALU.mult),
                     reads=[pbk, "gbc"], writes=[pbk])
                P.op("dve", lambda e, pb=pb, half=half, xbuf=xbuf: e.scalar_tensor_tensor(
                    out=xbuf[:, half * 512:(half + 1) * 512], in0=pb[:], scalar=sm[:, 7:8],
                    in1=xbuf[:, half * 512:(half + 1) * 512], op0=ALU.mult, op1=ALU.add),
                     reads=[pbk, "sm7", xk], writes=[xk])
            P.op("sp", lambda e, xbuf=xbuf, i=i: e.dma_start(out=out[i * 128:(i + 1) * 128, :], in_=xbuf[:]),
                 reads=[xk], writes=["h1row%d" % i], dma="h1st%d" % (i % 4))
            yield

        def merge(gens):
            gens = [[g, 0, max(1, n)] for g, n in gens]
            live = list(gens)
            while live:
                live.sort(key=lambda t: (t[1] + 1) / t[2])
                t = live[0]
                try:
                    next(t[0])
                    t[1] += 1
                except StopIteration:
                    live.remove(t)

        def run_all(g):
            for _ in g:
                pass

        def front_units(i):
            return 13

        def front_index(i):
            for _ in front(i):
                yield
            for _ in indexmm(i):
                yield

        run_all(front_index(0))
        run_all(chain(0))
        mask_tr(0)
        if NT > 1:
            run_all(front_index(1))
        for i_ in range(NT):
            gens = [(attn(i_), i_ + 5)]
            if i_ + 1 < NT:
                gens.append((chain(i_ + 1), int((NB + 2) * 1.4)))
            if i_ + 2 < NT:
                gens.append((front_index(i_ + 2), 13 + (i_ + 6) // 4))
            merge(gens)
            if i_ + 1 < NT:
                mask_tr(i_ + 1)
        if not do_ffn:
            P.final_wait("sp", ["h1st0", "h1st1", "h1st2", "h1st3"])
        P.barrier()
        P.emit()
        es1.close()

    if do_ffn:
        es2 = es
        gbc = sb("gbc2", [128, 2, D])
        P.op("sp", lambda e: e.dma_start(out=gbc[:].rearrange("p g d -> p (g d)"),
                                         in_=gains[2:4, :].rearrange("g d -> (g d)").partition_broadcast(128)),
             writes=["gbc"], dma="c4")
        w_up_sb = sb("w_up_sb", [128, KD, 2 * D_FF], BF16)
        w_down_sb = sb("w_down_sb", [128, NFC, D], BF16)
        cw = sb("cw", [128, 2 * NFC, 4])
        h1b = sb("h1b", [128, 2, D])
        hn_bf = sb("hn_bf", [128, D], BF16)
        junk = sb("junk", [128, D], BF16)
        hnT = sb("hnT", [128, KD, TS2], BF16)
        upb = [sb("upb%d" % i, [128, TS2 + 2]) for i in range(4)]
        yb = [sb("yb%d" % i, [128, TS2]) for i in range(4)]
        sgb = [sb("sgb%d" % i, [128, TS2]) for i in range(2)]
        actT = sb("actT", [128, NFC, TS2], BF16)
        carry = sb("carry", [128, 2 * NFC, 2])
        ssb = sb("ssb", [128, 8])
        tmpf = sb("tmpf", [128, D])

        P.op("sp", lambda e: e.dma_start(out=cw[:], in_=cwb), writes=["cw"], dma="c2")
        P.op("pool", lambda e: e.memset(carry[:], 0.0), writes=["carry"])
        wup_v = w_up.rearrange("(kc p) f -> p kc f", p=128)
        for c4 in range(0, NFC, 4):
            n4 = min(4, NFC - c4)
            for base in (0, NFC):
                f0, f1 = (base + c4) * 128, (base + c4 + n4) * 128
                P.op("pool", lambda e, f0=f0, f1=f1: e.dma_start(out=w_up_sb[:, :, f0:f1], in_=wup_v[:, :, f0:f1]),
                     writes=["wup%d" % fc for fc in range(base + c4, base + c4 + n4)],
                     dma="wup%d_%d" % (c4 // 4, base))
        wdn_v = w_down.rearrange("(fc p) n -> p fc n", p=128)
        for c in range(0, NFC, 2):
            P.op("pool", lambda e, c=c: e.dma_start(out=w_down_sb[:, c:c + 2, :], in_=wdn_v[:, c:c + 2, :]),
                 writes=["wdn%d" % c, "wdn%d" % (c + 1)], dma="wdn%d" % (c // 2))

        h1b2 = [h1b, sb("h1b_b", [128, 2, D])]
        hnT2 = [hnT, sb("hnT_b", [128, KD, TS2], BF16)]
        hn2 = [hn_bf, sb("hn_bf_b", [128, D], BF16)]

        def ffn_norm(st):
            h1b = h1b2[st % 2]
            for sub in range(2):
                hn_bf = hn2[sub]
                hnk = "hn_bf%d" % sub
                r0 = (st * 2 + sub) * 128
                hk = "h1b%d_%d" % (st % 2, sub)
                P.op("sp", lambda e, sub=sub, r0=r0, h1b=h1b: e.dma_start(out=h1b[:, sub, :], in_=h1_src[r0:r0 + 128, :]),
                     reads=["h1row%d" % (st * 2 + sub)], writes=[hk], dma="h1ld%d_%d" % (st % 2, sub))
                P.op("act", lambda e, sub=sub, h1b=h1b: e.activation(out=junk[:], in_=h1b[:, sub, :], func=AF.Square,
                                                            accum_out=ssb[:, 0:1]),
                     reads=[hk], writes=["junk", "ss0"])
                rstd_from_ss(ssb[:, 0:1], ssb[:, 2:3], ssb[:, 1:2], "ss0", "ss1", "ss2", 1, 1.0 / D)
                P.op("dve", lambda e, sub=sub, hn_bf=hn_bf, h1b=h1b: e.scalar_tensor_tensor(
                    out=hn_bf[:], in0=h1b[:, sub, :], scalar=ssb[:, 2:3], in1=gbc[:, 0, :],
                    op0=ALU.mult, op1=ALU.mult), reads=[hk, "ss2", "gbc"], writes=[hnk])

        def ffn_tr(st):
            hnT = hnT2[st % 2]
            hnTk = "hnT%d" % (st % 2)
            for sub in range(2):
                hn_bf = hn2[sub]
                hnk = "hn_bf%d" % sub
                pa = psAv[sub]
                pk = "psA%d" % sub
                for kc in range(KD):
                    P.op("pe", lambda e, kc=kc, pa=pa, hn_bf=hn_bf: e.transpose(out=pa[:, kc * 128:(kc + 1) * 128],
                                                                  in_=hn_bf[:, kc * 128:(kc + 1) * 128],
                                                                  identity=ident[:]),
                         reads=[hnk, "ident"], writes=[pk])
                P.op("act", lambda e, sub=sub, pa=pa, hnT=hnT: e.copy(
                    out=hnT[:, :, sub * 128:(sub + 1) * 128],
                    in_=pa.rearrange("p (k t) -> p k t", k=KD)), reads=[pk], writes=[hnTk])

        def ffn_up(st):
            hnT = hnT2[st % 2]
            hnTk = "hnT%d" % (st % 2)
            for c in range(NFC):
                ybs = []
                for gi, fc in enumerate((c, c + NFC)):
                    bi = (c % 2) * 2 + gi
                    pb = psB[gi] if c % 2 == 0 else psC[gi]
                    pbk = ("psB%d" if c % 2 == 0 else "psC%d") % gi
                    ub, ubk = upb[bi], "upb%d" % bi
                    y, yk = yb[bi], "yb%d" % bi
                    for kc in range(KD):
                        P.op("pe", lambda e, kc=kc, fc=fc, pb=pb, hnT=hnT: e.matmul(
                            pb[:, 0:TS2], lhsT=w_up_sb[:, kc, fc * 128:(fc + 1) * 128], rhs=hnT[:, kc, :],
                            start=(kc == 0), stop=(kc == KD - 1)),
                             reads=["wup%d" % fc, hnTk], writes=[pbk])
                    P.op("pool", lambda e, ub=ub, fc=fc: e.tensor_copy(out=ub[:, 0:2], in_=carry[:, fc, :]),
                         reads=["carry%d" % fc, "carry"], writes=[ubk])
                    P.op("act", lambda e, ub=ub, pb=pb: e.copy(out=ub[:, 2:TS2 + 2], in_=pb[:, 0:TS2]),
                         reads=[pbk], writes=[ubk])
                    P.op("act", lambda e, y=y, pb=pb, fc=fc: e.activation(
                        out=y[:], in_=pb[:, 0:TS2], func=AF.Identity, bias=cw[:, fc, 3:4], scale=cw[:, fc, 2:3]),
                         reads=[pbk, "cw"], writes=[yk])
                    P.op("pool", lambda e, ub=ub, fc=fc: e.tensor_copy(out=carry[:, fc, :], in_=ub[:, TS2:TS2 + 2]),
                         reads=[ubk], writes=["carry%d" % fc])
                    P.op("dve", lambda e, y=y, ub=ub, fc=fc: e.scalar_tensor_tensor(
                        out=y[:], in0=ub[:, 1:TS2 + 1], scalar=cw[:, fc, 1:2], in1=y[:], op0=ALU.mult, op1=ALU.add),
                         reads=[ubk, yk, "cw"], writes=[yk])
                    P.op("dve", lambda e, y=y, ub=ub, fc=fc: e.scalar_tensor_tensor(
                        out=y[:], in0=ub[:, 0:TS2], scalar=cw[:, fc, 0:1], in1=y[:], op0=ALU.mult, op1=ALU.add),
                         reads=[ubk, yk, "cw"], writes=[yk])
                    ybs.append((y, yk))
                sg, sgk = sgb[c % 2], "sgb%d" % (c % 2)
                P.op("act", lambda e, sg=sg, y=ybs[0][0]: e.activation(out=sg[:], in_=y[:], func=AF.Silu),
                     reads=[ybs[0][1]], writes=[sgk])
                P.op("dve", lambda e, sg=sg, y=ybs[1][0], c=c: e.tensor_tensor(
                    out=actT[:, c, :], in0=sg[:], in1=y[:], op=ALU.mult),
                     reads=[sgk, ybs[1][1]], writes=["actT%d" % c])
                if c == NFC // 2 and st + 1 < NST:
                    ffn_norm(st + 1)

        def ffn_down(st):
            h1b = h1b2[st % 2]
            for sub in range(2):
                r0 = (st * 2 + sub) * 128
                hk = "h1b%d_%d" % (st % 2, sub)
                pds = [(psD[0], "psD0"), (psD[1], "psD1")] if sub == 0 else [(psB[0], "psB0"), (psB[1], "psB1")]
                for half in range(2):
                    pd = pds[half][0]
                    for c in range(NFC):
                        P.op("pe", lambda e, c=c, pd=pd, sub=sub, half=half: e.matmul(
                            pd[:], lhsT=actT[:, c, sub * 128:(sub + 1) * 128],
                            rhs=w_down_sb[:, c, half * 512:(half + 1) * 512],
                            start=(c == 0), stop=(c == NFC - 1)),
                             reads=["actT%d" % c, "wdn%d" % c], writes=[pds[half][1]])
                for half in range(2):
                    P.op("act", lambda e, half=half, pd=pds[half][0]: e.activation(
                        out=junk[:, half * 512:(half + 1) * 512], in_=pd[:], func=AF.Square,
                        accum_out=ssb[:, 4 + half:5 + half]),
                         reads=[pds[half][1]], writes=["junk", "ssd%d" % half])
                P.op("pool", lambda e: e.tensor_tensor(out=ssb[:, 3:4], in0=ssb[:, 4:5], in1=ssb[:, 5:6], op=ALU.add),
                     reads=["ssd0", "ssd1"], writes=["ss3"])
                rstd_from_ss(ssb[:, 3:4], ssb[:, 7:8], ssb[:, 6:7], "ss3", "ss6", "ss7", 1, 1.0 / D)
                for half in range(2):
                    P.op("dve", lambda e, half=half, pd=pds[half][0]: e.scalar_tensor_tensor(
                        out=tmpf[:, half * 512:(half + 1) * 512], in0=pd[:], scalar=ssb[:, 7:8],
                        in1=gbc[:, 1, half * 512:(half + 1) * 512], op0=ALU.mult, op1=ALU.mult),
                         reads=[pds[half][1], "ss7", "gbc"], writes=["tmpf%d" % half])
                P.op("pool", lambda e, sub=sub, h1b=h1b: e.tensor_tensor(out=h1b[:, sub, :], in0=tmpf[:], in1=h1b[:, sub, :],
                                                                op=ALU.add),
                     reads=["tmpf0", "tmpf1", hk], writes=[hk])
                P.op("sp", lambda e, sub=sub, r0=r0, h1b=h1b: e.dma_start(out=out[r0:r0 + 128, :], in_=h1b[:, sub, :]),
                     reads=[hk], writes=["h1row%d" % (st * 2 + sub)], dma="ost%d_%d" % (st % 2, sub))

        ffn_norm(0)
        ffn_tr(0)
        for st in range(NST):
            ffn_up(st)
            if st + 1 < NST:
                ffn_tr(st + 1)
            ffn_down(st)
        P.final_wait("sp", ["ost0_0", "ost0_1", "ost1_0", "ost1_1"])
        P.emit()
    es.close()
    return nc


def host_consts(S):
    NT = S // 128
    pos = np.arange(S, dtype=np.float32)
    inv = (10000.0 ** (-np.arange(0, 64, 2, dtype=np.float32) / 64.0)).astype(np.float32)
    ang = pos[:, None] * inv[None, :]
    cs = np.concatenate([np.cos(ang), np.sin(ang)], axis=1).astype(np.float32)
    cs_tab = np.ascontiguousarray(cs.reshape(NT, 128, 64).transpose(1, 0, 2))
    consts = np.zeros((128, 512), np.float64)
    consts[:, 0:128] = np.eye(128)
    p = np.arange(128)
    consts[:, 128:256] = (p[None, :] >= p[:, None]).astype(np.float64)
    consts[:, 256:384] = np.where(p[None, :] <= p[:, None], 0.0, -1e30)
    g = 1.0 - 2.0 ** (-5.0 - np.arange(8))
    consts[:, 384:392] = g[None, :] ** (-(p[:, None] + 1.0)) * 0.125
    hpp = np.array([2 * (k % 4) + (k // 4) for k in range(8)])
    xi = g[None, hpp] ** (p[:, None] + 1.0)
    consts[:, 392:400] = xi * xi / 64.0
    consts[:, 400:408] = xi * 0.5
    for pr in range(4):
        consts[:, 408 + pr] = np.where(p < 64, g[2 * pr] ** 128.0, g[2 * pr + 1] ** 128.0)
    consts[:, 412:412 + N_BISECT] = 2.0 ** (-(np.arange(N_BISECT) + 1.0))[None, :]
    return cs_tab, consts.astype(np.float32)


def prep_inputs(b, S, x, mix_norm_pre, mix_norm_post, w_in, w_out, ffn_norm_pre, ffn_norm_post,
                w_up, conv_w, conv_b, w_down, shared):
    return dict(shared, x=np.ascontiguousarray(x[b, :S]))


def prep_shared(S, mix_norm_pre, mix_norm_post, w_in, w_out, ffn_norm_pre, ffn_norm_post,
                w_up, conv_w, conv_b, w_down):
    cs_tab, consts = host_consts(S)
    segs = [(0, 512), (512, 1024), (2048, 2560), (2688, 3200), (2560, 2624), (3200, 3264),
            (1024, 1536), (1536, 2048), (2624, 2688), (3264, 3272)]
    w_in_p = np.concatenate([w_in[0][:, a:b] for a, b in segs], axis=1)
    cwb = np.empty((128, 2 * NFC, 4), np.float32)
    cwb[:, :, 0:3] = conv_w[0].reshape(3, 2 * NFC, 128).transpose(2, 1, 0)
    cwb[:, :, 3] = conv_b[0].reshape(2 * NFC, 128).T
    gains = np.stack([mix_norm_pre[0], mix_norm_post[0], ffn_norm_pre[0], ffn_norm_post[0]], axis=0)
    consts = consts.copy()
    consts[:, 440:448] = mix_norm_pre[0].reshape(8, 128).T
    return dict(w_in=np.ascontiguousarray(w_in_p, dtype=np.float32), w_out=np.ascontiguousarray(w_out[0]),
                w_up=np.ascontiguousarray(w_up[0]), w_down=np.ascontiguousarray(w_down[0]),
                cwb=cwb, gains=np.ascontiguousarray(gains, dtype=np.float32), cs_tab=cs_tab, consts=consts)


def kernel(**inputs):
    inputs = {k: np.asarray(v) for k, v in inputs.items()}
    x = inputs["x"]
    B, S, _ = x.shape
    nc = build_nc(S)
    wkeys = ["mix_norm_pre", "mix_norm_post", "w_in", "w_out", "ffn_norm_pre", "ffn_norm_post",
             "w_up", "conv_w", "conv_b", "w_down"]
    shared = prep_shared(S, *[inputs[k] for k in wkeys])
    in_maps = [dict(shared, x=np.ascontiguousarray(x[b])) for b in range(B)]
    res = run_bass_kernel_spmd(nc, in_maps, core_ids=list(range(B)))
    return np.stack([np.asarray(r["out"]) for r in res.results], axis=0).astype(np.float32)
```
